# Optimizing a Trainium2 kernel written in Bass

```python
import math
import jax, jax.numpy as jnp
from jax import lax
import numpy as np

D_MODEL = 1024
BATCH = 2
SEQ = 8192
DEPTH = 2

N_EVEN = (DEPTH + 1) // 2
N_ODD = DEPTH // 2

D_A = D_MODEL // 2
D_B = D_MODEL // 2
CONV_A = 31
CONV_B = 3
D_IN_EVEN = 2 * D_A + 3 * D_B

D_RNN = D_MODEL
RNN_BLOCK = 256
RNN_HEADS = D_RNN // RNN_BLOCK
CONV_C = 4
LRU_C = 8.0

N_EXPERTS = 16
N_EXPERT_GROUPS = 4
EXPERTS_PER_GROUP = N_EXPERTS // N_EXPERT_GROUPS
TOP_K = 2
D_EXPERT = D_MODEL
MOE_BLOCK = 128

LN_EPS = 1e-5
ALPHA = (2.0 * DEPTH) ** 0.25
BETA = (8.0 * DEPTH) ** -0.25

kernel_name = "hybrid_conformer_shortconv_rglru_grouped_moe_deepnorm"


def layer_norm(x, g, b):
    xf = x.astype(jnp.float32)
    mu = jnp.mean(xf, axis=-1, keepdims=True)
    var = jnp.mean(jnp.square(xf - mu), axis=-1, keepdims=True)
    y = (xf - mu) * lax.rsqrt(var + LN_EPS)
    return (y * g.astype(jnp.float32) + b.astype(jnp.float32)).astype(x.dtype)


def causal_depthwise_conv(x, w):
    k, c = w.shape
    return lax.conv_general_dilated(
        x, w[:, None, :].astype(x.dtype), window_strides=(1,), padding=[(k - 1, 0)],
        dimension_numbers=("NWC", "WIO", "NWC"), feature_group_count=c)


def conv_mixer(x, w_in, a_dw, a_dw_b, a_ln_g, a_ln_b, b_dw, w_out):
    u = jnp.einsum("bsd,de->bse", x, w_in)
    a_val, a_gate, b_b, b_c, b_x = jnp.split(
        u, [D_A, 2 * D_A, 2 * D_A + D_B, 2 * D_A + 2 * D_B], axis=-1)
    a = a_val * jax.nn.sigmoid(a_gate)
    a = causal_depthwise_conv(a, a_dw) + a_dw_b
    a = jax.nn.silu(layer_norm(a, a_ln_g, a_ln_b))
    bb = b_b * causal_depthwise_conv(b_c * b_x, b_dw)
    return jnp.einsum("bse,ed->bsd", jnp.concatenate([a, bb], axis=-1), w_out)


def _linear_recurrence_combine(c1, c2):
    a1, b1 = c1
    a2, b2 = c2
    return a1 * a2, a2 * b1 + b2


def rglru_mixer(x, w_in, c_dw, c_dw_b, w_gate_a, b_gate_a, w_gate_x, b_gate_x, lam, w_out):
    bsz, seq, _ = x.shape
    u = jnp.einsum("bsd,de->bse", x, w_in)
    y_branch, xr = jnp.split(u, 2, axis=-1)
    xr = causal_depthwise_conv(xr, c_dw) + c_dw_b
    xh = xr.reshape(bsz, seq, RNN_HEADS, RNN_BLOCK)
    r = jax.nn.sigmoid(jnp.einsum("bshi,hij->bshj", xh, w_gate_a) + b_gate_a)
    i = jax.nn.sigmoid(jnp.einsum("bshi,hij->bshj", xh, w_gate_x) + b_gate_x)
    r = r.reshape(bsz, seq, D_RNN).astype(jnp.float32)
    i = i.reshape(bsz, seq, D_RNN)
    log_a = -LRU_C * r * jax.nn.softplus(-lam.astype(jnp.float32))
    a = jnp.exp(log_a)
    mult = jnp.sqrt(-jnp.expm1(2.0 * log_a))
    b = mult * (i * xr).astype(jnp.float32)
    _, h = lax.associative_scan(_linear_recurrence_combine, (a, b), axis=1)
    out = jax.nn.gelu(y_branch) * h.astype(x.dtype)
    return jnp.einsum("bse,ed->bsd", out, w_out)


def route(xf, w_router, b_router):
    t = xf.shape[0]
    logits = (xf @ w_router + b_router).astype(jnp.float32)
    probs = jax.nn.softmax(logits, axis=-1)
    pg = probs.reshape(t, N_EXPERT_GROUPS, EXPERTS_PER_GROUP)
    group_score = lax.top_k(pg, TOP_K)[0].sum(-1)
    g_sel = jnp.argmax(group_score, axis=-1)
    p_in = jnp.take_along_axis(pg, g_sel[:, None, None], axis=1)[:, 0]
    top_p, top_i = lax.top_k(p_in, TOP_K)
    expert_idx = g_sel[:, None] * EXPERTS_PER_GROUP + top_i
    gates = top_p / jnp.sum(top_p, axis=-1, keepdims=True)
    return expert_idx, gates


def moe(x, w_router, b_router, w1, w3, w2):
    bsz, seq, d = x.shape
    t = bsz * seq
    xf = x.reshape(t, d)
    e_idx, gates = route(xf, w_router, b_router)
    n_assign = t * TOP_K
    e_flat = e_idx.reshape(n_assign)
    tok_flat = jnp.repeat(jnp.arange(t, dtype=jnp.int32), TOP_K)
    g_flat = gates.reshape(n_assign)
    order = jnp.argsort(e_flat)
    e_s, tok_s, g_s = e_flat[order], tok_flat[order], g_flat[order]
    counts = jnp.bincount(e_flat, length=N_EXPERTS)
    padded = (counts + MOE_BLOCK - 1) // MOE_BLOCK * MOE_BLOCK
    start = jnp.cumsum(counts) - counts
    pend = jnp.cumsum(padded)
    pstart = pend - padded
    dest = pstart[e_s] + jnp.arange(n_assign, dtype=jnp.int32) - start[e_s]
    n_blocks = -(-n_assign // MOE_BLOCK) + N_EXPERTS
    n_pad = n_blocks * MOE_BLOCK
    x_pad = jnp.zeros((n_pad, d), x.dtype).at[dest].set(xf[tok_s])
    block_expert = jnp.minimum(
        jnp.searchsorted(pend, jnp.arange(n_blocks, dtype=pend.dtype) * MOE_BLOCK, side="right"),
        N_EXPERTS - 1)

    def expert_block(args):
        xb, e = args
        hb = jax.nn.silu(xb @ w1[e]) * (xb @ w3[e])
        return hb @ w2[e]

    y_pad = lax.map(expert_block, (x_pad.reshape(n_blocks, MOE_BLOCK, d), block_expert))
    y = y_pad.reshape(n_pad, d)[dest] * g_s[:, None].astype(x.dtype)
    out = jnp.zeros((t, d), x.dtype).at[tok_s].add(y)
    return out.reshape(bsz, seq, d)


def setup_inputs(seed: int = 0) -> dict:
    key = jax.random.key(seed)
    ks = jax.random.split(key, 26)
    nrm = lambda k, shape, s: jax.random.normal(k, shape, jnp.float32) * s
    a0 = jax.random.uniform(ks[19], (N_ODD, D_RNN), jnp.float32, 0.9, 0.999)
    s0 = a0 ** (1.0 / LRU_C)
    lam = jnp.log(s0) - jnp.log1p(-s0)
    return {
        "x": nrm(ks[0], (BATCH, SEQ, D_MODEL), 1.0),
        "ln1_g": 1.0 + nrm(ks[1], (DEPTH, D_MODEL), 0.02),
        "ln1_b": nrm(ks[2], (DEPTH, D_MODEL), 0.02),
        "ln2_g": 1.0 + nrm(ks[3], (DEPTH, D_MODEL), 0.02),
        "ln2_b": nrm(ks[4], (DEPTH, D_MODEL), 0.02),
        "even_w_in": nrm(ks[5], (N_EVEN, D_MODEL, D_IN_EVEN), D_MODEL ** -0.5),
        "even_a_dw": nrm(ks[6], (N_EVEN, CONV_A, D_A), CONV_A ** -0.5),
        "even_a_dw_b": nrm(ks[7], (N_EVEN, D_A), 0.02),
        "even_a_ln_g": 1.0 + nrm(ks[8], (N_EVEN, D_A), 0.02),
        "even_a_ln_b": nrm(ks[9], (N_EVEN, D_A), 0.02),
        "even_b_dw": nrm(ks[10], (N_EVEN, CONV_B, D_B), CONV_B ** -0.5),
        "even_w_out": nrm(ks[11], (N_EVEN, D_A + D_B, D_MODEL), BETA * (D_A + D_B) ** -0.5),
        "odd_w_in": nrm(ks[12], (N_ODD, D_MODEL, 2 * D_RNN), D_MODEL ** -0.5),
        "odd_c_dw": nrm(ks[13], (N_ODD, CONV_C, D_RNN), CONV_C ** -0.5),
        "odd_c_dw_b": nrm(ks[14], (N_ODD, D_RNN), 0.02),
        "odd_w_gate_a": nrm(ks[15], (N_ODD, RNN_HEADS, RNN_BLOCK, RNN_BLOCK), RNN_BLOCK ** -0.5),
        "odd_b_gate_a": nrm(ks[16], (N_ODD, RNN_HEADS, RNN_BLOCK), 0.02),
        "odd_w_gate_x": nrm(ks[17], (N_ODD, RNN_HEADS, RNN_BLOCK, RNN_BLOCK), RNN_BLOCK ** -0.5),
        "odd_b_gate_x": nrm(ks[18], (N_ODD, RNN_HEADS, RNN_BLOCK), 0.02),
        "odd_lam": lam,
        "odd_w_out": nrm(ks[20], (N_ODD, D_RNN, D_MODEL), BETA * D_RNN ** -0.5),
        "w_router": nrm(ks[21], (D_MODEL, N_EXPERTS), D_MODEL ** -0.5),
        "b_router": nrm(ks[22], (N_EXPERTS,), 0.01),
        "moe_w1": nrm(ks[23], (DEPTH, N_EXPERTS, D_MODEL, D_EXPERT), D_MODEL ** -0.5),
        "moe_w3": nrm(ks[24], (DEPTH, N_EXPERTS, D_MODEL, D_EXPERT), D_MODEL ** -0.5),
        "moe_w2": nrm(ks[25], (DEPTH, N_EXPERTS, D_EXPERT, D_MODEL), BETA * D_EXPERT ** -0.5),
    }


def reference(x, ln1_g, ln1_b, ln2_g, ln2_b, even_w_in, even_a_dw, even_a_dw_b, even_a_ln_g,
              even_a_ln_b, even_b_dw, even_w_out, odd_w_in, odd_c_dw, odd_c_dw_b, odd_w_gate_a,
              odd_b_gate_a, odd_w_gate_x, odd_b_gate_x, odd_lam, odd_w_out, w_router, b_router,
              moe_w1, moe_w3, moe_w2):
    for layer in range(DEPTH):
        j = layer // 2
        if layer % 2 == 0:
            m = conv_mixer(x, even_w_in[j], even_a_dw[j], even_a_dw_b[j], even_a_ln_g[j],
                           even_a_ln_b[j], even_b_dw[j], even_w_out[j])
        else:
            m = rglru_mixer(x, odd_w_in[j], odd_c_dw[j], odd_c_dw_b[j], odd_w_gate_a[j],
                            odd_b_gate_a[j], odd_w_gate_x[j], odd_b_gate_x[j], odd_lam[j],
                            odd_w_out[j])
        x = layer_norm(ALPHA * x + m, ln1_g[layer], ln1_b[layer])
        f = moe(x, w_router, b_router, moe_w1[layer], moe_w3[layer], moe_w2[layer])
        x = layer_norm(ALPHA * x + f, ln2_g[layer], ln2_b[layer])
    return x
```

```python
from contextlib import ExitStack
import numpy as np
import concourse.bass as bass
import concourse.mybir as mybir
from concourse.bass_utils import run_bass_kernel_spmd

F32 = mybir.dt.float32
BF16 = mybir.dt.bfloat16
AF = mybir.ActivationFunctionType
ALU = mybir.AluOpType
AX = mybir.AxisListType

ENGS = ["pe", "act", "dve", "pool", "sp"]
NCORES = 8
T = 2048
NT = 4
TW = 512
D = 1024
KC = 8
HALO = 32
DEPTH = 2
ALPHA = (2.0 * DEPTH) ** 0.25
LN_EPS = 1e-5
NE = 16
RING = 7
DBG = {"lvl": 9}


class Sched:
    def __init__(self, nc):
        self.nc = nc
        self.prog = {e: [] for e in ENGS}
        self.cnt = {e: 0 for e in ENGS}
        self.seen = {e: {} for e in ENGS}
        self.lastw = {}
        self.reads = {}
        self.dma_cnt = {}
        self.same = {"pool", "dve", "act"}

    def _deps(self, rd, wr):
        deps = []
        for r in rd:
            t = self.lastw.get(r)
            if t is not None:
                deps.append(t)
        for w in wr:
            t = self.lastw.get(w)
            if t is not None:
                deps.append(t)
            deps.extend(self.reads.get(w, ()))
        return deps

    def _filter(self, eng, deps):
        need = {}
        for sk, v in deps:
            if sk == ("eng", eng) and eng not in self.same:
                continue
            if self.seen[eng].get(sk, 0) >= v:
                continue
            if need.get(sk, 0) < v:
                need[sk] = v
        for sk, v in need.items():
            self.seen[eng][sk] = v
        return list(need.items())

    def _commit(self, tick, rd, wr):
        for r in rd:
            self.reads.setdefault(r, []).append(tick)
        for w in wr:
            self.lastw[w] = tick
            self.reads[w] = []

    def op(self, eng, fns, rd=(), wr=()):
        if callable(fns):
            fns = [fns]
        if eng != "pe" and len(fns) > 1:
            for fn in fns:
                tick = self.op(eng, fn, rd, wr)
            return tick
        px = [("psx", r[1]) for r in rd if isinstance(r, tuple) and r[0] == "ps"]
        if px:
            wr = list(wr) + px
        waits = self._filter(eng, self._deps(rd, wr))
        self.cnt[eng] += 1
        sk = ("eng", eng)
        tick = (sk, self.cnt[eng])
        self.prog[eng].append((waits, fns, sk, 1))
        self._commit(tick, rd, wr)
        return tick

    def dma(self, q, fn, semkey, rd=(), wr=()):
        waits = self._filter(q, self._deps(rd, wr))
        sk = ("dma", semkey)
        self.dma_cnt[sk] = self.dma_cnt.get(sk, 0) + 16
        tick = (sk, self.dma_cnt[sk])
        self.prog[q].append((waits, [fn], sk, 16))
        self._commit(tick, rd, wr)
        return tick

    def wait_res(self, eng, res_list):
        deps = []
        for r in res_list:
            t = self.lastw.get(r)
            if t is not None:
                deps.append(t)
            deps.extend(self.reads.get(r, ()))
        waits = self._filter(eng, deps)
        self.prog[eng].append((waits, [], None, 0))

    def barrier(self):
        targets = [(("eng", e), self.cnt[e]) for e in ENGS if self.cnt[e] > 0] + list(self.dma_cnt.items())
        for e in ENGS:
            waits = self._filter(e, targets)
            self.prog[e].append((waits, [], None, 0))

    def emit(self, stack):
        nc = self.nc
        sems = {}
        keys = [("eng", e) for e in ENGS] + list(self.dma_cnt.keys())
        for i, k in enumerate(keys):
            sems[k] = stack.enter_context(nc.semaphore("s%d" % i))
        block = stack.enter_context(nc.Block())
        prog = self.prog

        def run(engname):
            def body(eng):
                for waits, fns, sk, inc in prog[engname]:
                    for wk, wv in waits:
                        eng.wait_ge(sems[wk], wv)
                    for i, fn in enumerate(fns):
                        ins = fn(eng)
                        if i == len(fns) - 1:
                            ins.then_inc(sems[sk], inc)
            return body

        block.tensor(run("pe"))
        block.scalar(run("act"))
        block.vector(run("dve"))
        block.gpsimd(run("pool"))
        block.sync(run("sp"))


def _pvec_layout():
    off = {}
    n = 0

    def add(name, w):
        nonlocal n
        off[name] = (n, w)
        n += w

    add("a_dw", 4 * 31)
    add("a_dw_b", 4)
    add("a_ln_g", 4)
    add("a_ln_b", 4)
    add("b_dw", 4 * 3)
    for l in range(DEPTH):
        add("ln1_g%d" % l, 8)
        add("ln1_b%d" % l, 8)
        add("ln2_g%d" % l, 8)
        add("ln2_b%d" % l, 8)
    add("c_dw", 8 * 4)
    add("c_dw_b", 8)
    add("b_ga", 8)
    add("b_gx", 8)
    add("lam", 8)
    add("b_router", 16)
    add("mrank", 8)
    add("mrank_c", 8)
    add("hsel", 8)
    add("cmask", 4)
    return off, n


PV_OFF, NV = _pvec_layout()


def _chunks(v, nchunk):
    return np.ascontiguousarray(v.reshape(nchunk, 128).T)


def pack_pvec(inp, core):
    pv = np.zeros((128, NV), np.float32)

    def put(name, arr):
        o, w = PV_OFF[name]
        pv[:, o:o + w] = arr.reshape(128, w)

    a_dw = inp["even_a_dw"][0]
    put("a_dw", np.ascontiguousarray(a_dw.reshape(31, 4, 128).transpose(2, 1, 0)))
    put("a_dw_b", _chunks(inp["even_a_dw_b"][0], 4))
    put("a_ln_g", _chunks(inp["even_a_ln_g"][0], 4))
    put("a_ln_b", _chunks(inp["even_a_ln_b"][0], 4))
    put("b_dw", np.ascontiguousarray(inp["even_b_dw"][0].reshape(3, 4, 128).transpose(2, 1, 0)))
    for l in range(DEPTH):
        put("ln1_g%d" % l, _chunks(inp["ln1_g"][l], 8))
        put("ln1_b%d" % l, _chunks(inp["ln1_b"][l], 8))
        put("ln2_g%d" % l, _chunks(inp["ln2_g"][l], 8))
        put("ln2_b%d" % l, _chunks(inp["ln2_b"][l], 8))
    put("c_dw", np.ascontiguousarray(inp["odd_c_dw"][0].reshape(4, 8, 128).transpose(2, 1, 0)))
    put("c_dw_b", _chunks(inp["odd_c_dw_b"][0], 8))
    put("b_ga", _chunks(inp["odd_b_gate_a"][0].reshape(-1), 8))
    put("b_gx", _chunks(inp["odd_b_gate_x"][0].reshape(-1), 8))
    put("lam", _chunks(inp["odd_lam"][0], 8))
    put("b_router", np.broadcast_to(inp["b_router"][None, :], (128, 16)))
    m = np.zeros(8, np.float32)
    b0 = (core // 4) * 4
    m[b0:core] = 1.0
    put("mrank", np.broadcast_to(m[None, :], (128, 8)))
    put("mrank_c", np.broadcast_to((1.0 - m)[None, :], (128, 8)))
    hs = np.zeros(8, np.float32)
    if core % 4 != 0:
        hs[core - 1] = 1.0
    put("hsel", np.broadcast_to(hs[None, :], (128, 8)))
    cm = np.array([1.0 if (core % 4) - 3 + j >= 0 else 0.0 for j in range(4)], np.float32)
    put("cmask", np.broadcast_to(cm[None, :], (128, 4)))
    return pv


class K:
    pass


def build_program(stage="full"):
    nc = bass.Bass("TRN2", target_bir_lowering=False)
    g = K()
    g.nc = nc
    g.uid = 0
    _orig_sbuf = nc.sbuf_tensor

    def _sbuf(name, shape, dtype, **kw):
        g.uid += 1
        return _orig_sbuf("%s_u%d" % (name, g.uid), shape, dtype, **kw)
    g.sbuf = _sbuf

    def din(name, shape):
        return nc.dram_tensor(name, list(shape), F32, kind="ExternalInput").ap()

    g.xin = din("xin", [(4 * T if stage == "fused" else T) + HALO, D])
    g.pvd = din("pvec", [128, NV])
    g.e_w_in = din("even_w_in", [D, 2560])
    g.e_w_out = din("even_w_out", [D, D])
    g.o_w_in = din("odd_w_in", [D, 2048])
    g.o_wga = din("odd_w_gate_a", [4, 256, 256])
    g.o_wgx = din("odd_w_gate_x", [4, 256, 256])
    g.o_w_out = din("odd_w_out", [D, D])
    g.w_router = din("w_router", [D, NE])
    g.w1, g.w3, g.w2 = {}, {}, {}
    for l in range(DEPTH):
        if (l == 0 and stage in ("full", "l0", "fused")) or (l == 1 and stage in ("full", "l1f", "fused")):
            g.w1[l] = din("moe_w1_%d" % l, [NE, D, D])
            g.w3[l] = din("moe_w3_%d" % l, [NE, D, D])
            g.w2[l] = din("moe_w2_%d" % l, [NE, D, D])
    if stage in ("l1f", "l1mix"):
        g.hp_all_d = din("hp_all", [128, NCORES, 16])
    if stage == "l1s":
        g.hp_out = nc.dram_tensor("hp", [128, 16], F32, kind="ExternalOutput").ap()
    else:
        g.out = nc.dram_tensor("out", [T, D], F32, kind="ExternalOutput").ap()
    with ExitStack() as st:
        s = Sched(nc)
        g.s = s
        g.xres = st.enter_context(nc.sbuf_tensor("xres", [128, KC, T], F32))
        g.pv = st.enter_context(nc.sbuf_tensor("pv", [128, NV], F32))
        g.ident = st.enter_context(nc.sbuf_tensor("ident", [128, 128], F32))
        g.ones1 = st.enter_context(nc.sbuf_tensor("ones1", [128, 128], F32))
        g.onesA = st.enter_context(nc.sbuf_tensor("onesA", [128, 128], F32))
        g.onesD = st.enter_context(nc.sbuf_tensor("onesD", [128, 128], F32))
        g.pvx = st.enter_context(nc.sbuf_tensor("pvx", [128, 64], F32))
        g.ps = [st.enter_context(nc.psum_tensor("ps%d" % i, [128, TW], F32)) for i in range(8)]
        g.bank_rr = {}

        setup_consts(g)
        if stage == "fused":
            g.keep = st.enter_context(nc.sbuf_tensor("keep", [128, 32], F32))
            s.op("dve", lambda e: e.memset(g.keep[:], 0.0), wr=["keep"])
            for j in range(4):
                mixer0(g, st, row0=j * T)
                s.barrier()
                moe(g, st, 0, final=False)
                s.barrier()
                if j < 3:
                    mixer1(g, st, "state", "sbuf", it=j)
                    s.barrier()
            mixer1(g, st, "full", "sbuf")
            s.barrier()
            moe(g, st, 1, final=True)
            s.barrier()
        if stage in ("full", "l0", "l0mix"):
            mixer0(g, st)
            s.barrier()
        if stage in ("full", "l0"):
            moe(g, st, 0, final=(stage == "l0"))
            s.barrier()
        if stage == "l1s":
            mixer1(g, st, "state", "dram")
        if stage in ("l1f", "l1mix"):
            mixer1(g, st, "full", "dram")
            s.barrier()
        if stage == "l1f":
            moe(g, st, 1, final=True)
            s.barrier()
        if stage != "l1s":
            write_out(g, st)
        s.emit(st)
    return nc


def pvs(g, name, i=None, w=1):
    o, n = PV_OFF[name]
    if i is None:
        return g.pv[:, o:o + n]
    return g.pv[:, o + i:o + i + w]


def next_bank(g, group, banks):
    i = g.bank_rr.get(group, 0)
    g.bank_rr[group] = i + 1
    b = banks[i % len(banks)]
    return b


def setup_consts(g):
    s = g.s
    s.dma("sp", lambda e: e.dma_start(out=g.pv[:], in_=g.pvd), "pv", wr=["pv"])
    s.op("pool", lambda e: e.memset(g.ones1[:], 1.0), wr=["ones1"])
    s.op("pool", lambda e: e.memset(g.onesA[:], 1.0 / 512.0), wr=["onesA"])
    s.op("pool", lambda e: e.memset(g.onesD[:], 1.0 / 1024.0), wr=["onesD"])
    s.op("pool", lambda e: e.affine_select(out=g.ident[:], in_=g.ones1[:], pattern=[[-1, 128]],
                                           compare_op=ALU.is_equal, fill=0.0, base=0, channel_multiplier=1),
         rd=["ones1"], wr=["ident"])
    for i, nm in enumerate(["ln1_g0", "ln1_b0", "ln2_g0", "ln2_b0", "ln1_g1", "ln1_b1"]):
        s.op("dve", lambda e, i=i, nm=nm: e.tensor_scalar(g.pvx[:, i * 8:(i + 1) * 8], pvs(g, nm), ALPHA, None, ALU.mult),
             rd=["pv"], wr=["pvx"])


def mm_group(g, out_ap, pairs, rd, wr):
    n = len(pairs)
    fns = []
    for i, (l, r) in enumerate(pairs):
        fns.append(lambda e, l=l, r=r, i=i: e.matmul(out_ap, lhsT=l, rhs=r, start=(i == 0), stop=(i == n - 1)))
    return g.s.op("pe", fns, rd=rd, wr=wr)


def layer_norm_tile(g, t, nch, src_fn, src_res_fn, ones, ones_res, out_fn, tmp, stat_banks, func=AF.Identity):
    s = g.s
    nb = tmp["nb"]
    epsap = g.epsc[:, 0:1]
    bm, bx = stat_banks
    bmk, bxk = ("ps", bm), ("ps", bx)
    pm, px = g.ps[bm], g.ps[bx]
    for c in range(nch):
        sq = tmp["sq"][:, c % nb, :]
        s.op("act", lambda e, c=c, sq=sq: e.activation(out=sq, in_=src_fn(c), func=AF.Square),
             rd=[src_res_fn(c)], wr=[("lnsq", c % nb)])
        s.op("pe", lambda e, c=c: e.matmul(pm[:], lhsT=ones[:], rhs=src_fn(c), start=(c == 0), stop=(c == nch - 1)),
             rd=[src_res_fn(c), ones_res], wr=[bmk])
        s.op("pe", lambda e, c=c, sq=sq: e.matmul(px[:], lhsT=ones[:], rhs=sq, start=(c == 0), stop=(c == nch - 1)),
             rd=[("lnsq", c % nb)], wr=[bxk])
    s.op("act", lambda e: e.activation(out=tmp["msq"][:], in_=pm[:], func=AF.Square), rd=[bmk], wr=["lnmsq"])
    s.op("dve", lambda e: e.tensor_tensor(out=tmp["var"][:], in0=px[:], in1=tmp["msq"][:], op=ALU.subtract),
         rd=[bxk, "lnmsq"], wr=["lnvar"])
    s.op("act", lambda e: e.activation(out=tmp["var"][:], in_=tmp["var"][:], func=AF.Sqrt, bias=epsap),
         rd=["lnvar", "eps"], wr=["lnvar"])
    s.op("dve", lambda e: e.reciprocal(out=tmp["rstd"][:], in_=tmp["var"][:]), rd=["lnvar"], wr=["lnrstd"])
    for c in range(nch):
        t1 = tmp["t1"][:, c % nb, :]
        t2 = tmp["t2"][:, c % nb, :]
        s.op("dve", lambda e, c=c, t1=t1: e.tensor_tensor(out=t1, in0=src_fn(c), in1=pm[:], op=ALU.subtract),
             rd=[src_res_fn(c), bmk], wr=[("lnt1", c % nb)])
        s.op("dve", lambda e, t1=t1, t2=t2: e.tensor_tensor(out=t2, in0=t1, in1=tmp["rstd"][:], op=ALU.mult),
             rd=[("lnt1", c % nb), "lnrstd"], wr=[("lnt2", c % nb)])
        for (oap, ores, gap, bap) in out_fn(c):
            s.op("act", lambda e, oap=oap, t2=t2, gap=gap, bap=bap: e.activation(out=oap, in_=t2, func=func, bias=bap, scale=gap),
                 rd=[("lnt2", c % nb), "pv", "pvx"], wr=[ores])


def alloc_ln_tmp(g, st, tag, nb=2):
    nc = g.nc
    tmp = {}
    tmp["nb"] = nb
    tmp["sq"] = st.enter_context(g.sbuf("lnsq" + tag, [128, nb, TW], F32))
    tmp["msq"] = st.enter_context(g.sbuf("lnmsq" + tag, [128, TW], F32))
    tmp["var"] = st.enter_context(g.sbuf("lnvar" + tag, [128, TW], F32))
    tmp["rstd"] = st.enter_context(g.sbuf("lnrstd" + tag, [128, TW], F32))
    tmp["t1"] = st.enter_context(g.sbuf("lnt1" + tag, [128, nb, TW], F32))
    tmp["t2"] = st.enter_context(g.sbuf("lnt2" + tag, [128, nb, TW], F32))
    return tmp


def load_w_bf(g, dst, dst_res, src2d, ncols, semkey, colchunk=512, c0=0):
    s = g.s
    v = src2d.rearrange("(kc p) n -> p kc n", p=128)
    for cc in range(0, ncols, colchunk):
        w = min(colchunk, ncols - cc)
        s.dma("pool", lambda e, cc=cc, w=w: e.dma_start(out=dst[:, :, cc:cc + w], in_=v[:, :, c0 + cc:c0 + cc + w]),
              (semkey, cc // colchunk), wr=[(dst_res, cc // colchunk)])


def mixer0(g, st0, row0=0):
    nc, s = g.nc, g.s
    with ExitStack() as st:
        win = st.enter_context(g.sbuf("m0_win", [128, KC, 2560], BF16))
        wout = st.enter_context(g.sbuf("m0_wout", [128, KC, D], BF16))
        xt = st.enter_context(g.sbuf("m0_xt", [128, 4, D], F32))
        xh = xt[0:HALO, 0, :]
        xbt = st.enter_context(g.sbuf("m0_xbt", [128, KC, TW], BF16))
        xhb = st.enter_context(g.sbuf("m0_xhb", [128, KC, HALO], BF16))
        abuf = st.enter_context(g.sbuf("m0_abuf", [128, 4, 30 + TW], BF16))
        cxbuf = st.enter_context(g.sbuf("m0_cxbuf", [128, 4, 2 + TW], F32))
        cv = st.enter_context(g.sbuf("m0_cv", [128, 4, TW], F32))
        sig = st.enter_context(g.sbuf("m0_sig", [128, 1, TW], F32))
        bcs = st.enter_context(g.sbuf("m0_bcs", [128, 1, TW], F32))
        cb = st.enter_context(g.sbuf("m0_cb", [128, 2, TW], F32))
        mix = st.enter_context(g.sbuf("m0_mix", [128, KC, TW], BF16))
        g.epsc = st.enter_context(g.sbuf("m0_eps", [128, 1], F32))
        scrA = st.enter_context(g.sbuf("m0_scrA", [128, 4 * TW], F32))
        scrB = st.enter_context(g.sbuf("m0_scrB", [128, 4 * TW], F32))
        tmp = {"nb": 2,
               "t1": scrA[:, 0:2 * TW].rearrange("p (a b) -> p a b", b=TW),
               "t2": scrA[:, 2 * TW:4 * TW].rearrange("p (a b) -> p a b", b=TW),
               "sq": scrB[:, 0:2 * TW].rearrange("p (a b) -> p a b", b=TW),
               "msq": scrB[:, 2 * TW:3 * TW],
               "var": scrB[:, 3 * TW:4 * TW]}
        tmp["rstd"] = st.enter_context(g.sbuf("m0_rstd", [128, TW], F32))
        dgv = [scrA[:].bitcast(BF16).rearrange("p (k m) -> p k m", m=128), scrB[:].bitcast(BF16).rearrange("p (k m) -> p k m", m=128)]
        dg_alias = [[("lnt1", 0), ("lnt1", 1), ("lnt2", 0), ("lnt2", 1)], [("lnsq", 0), ("lnsq", 1), "lnmsq", "lnvar"]]
        s.op("pool", lambda e, ep=g.epsc: e.memset(ep[:], LN_EPS), wr=["eps"])

        lvl = DBG["lvl"]
        if lvl < 1:
            return
        if lvl >= 2:
            load_w_bf(g, win, "m0win", g.e_w_in, 2560, "m0win")
            load_w_bf(g, wout, "m0wout", g.e_w_out, D, "m0wout")
        winres = [("m0win", i) for i in range(5)]
        woutres = [("m0wout", i) for i in range(2)]
        PB = [0, 1, 2, 3, 4, 5]
        xin_t = g.xin[row0 + HALO:row0 + HALO + T, :].rearrange("(t q p) d -> t p q d", q=4, p=128)

        if lvl >= 3:
            s.dma("sp", lambda e: e.dma_start(out=xh, in_=g.xin[row0:row0 + HALO, :]), "m0xh", wr=["xt"])
        for c in range(KC if lvl >= 3 else 0):
            b = next_bank(g, "m0", PB)
            s.op("pe", lambda e, c=c, b=b: e.transpose(g.ps[b][:, 0:HALO], xh[:, c * 128:(c + 1) * 128], g.ident[0:HALO, 0:HALO]),
                 rd=["xt", "ident"], wr=[("ps", b)])
            s.op("dve", lambda e, c=c, b=b: e.tensor_copy(out=xhb[:, c, :], in_=g.ps[b][:, 0:HALO]), rd=[("ps", b)], wr=[("xhb", c)])
        xhb_res = [("xhb", c) for c in range(KC)]
        for j in range(4 if lvl >= 3 else 0):
            bk = {}
            for nm, strm in (("av", 0), ("ag", 1), ("bc", 3), ("bx", 4)):
                m = strm * 4 + j
                b = next_bank(g, "m0", PB)
                bk[nm] = b
                mm_group(g, g.ps[b][:, 0:HALO], [(win[:, k, m * 128:(m + 1) * 128], xhb[:, k, :]) for k in range(KC)],
                         rd=winres + xhb_res, wr=[("ps", b)])
            s.op("act", lambda e, b=bk["ag"], j=j: e.activation(out=sig[:, 0, 0:HALO], in_=g.ps[b][:, 0:HALO], func=AF.Sigmoid),
                 rd=[("ps", bk["ag"])], wr=[("sig", 0)])
            s.op("dve", lambda e, b=bk["av"], j=j: e.tensor_tensor(out=abuf[:, j, 0:30], in0=g.ps[b][:, 2:HALO], in1=sig[:, 0, 2:HALO], op=ALU.mult),
                 rd=[("ps", bk["av"]), ("sig", 0)], wr=[("abuf", j)])
            s.op("act", lambda e, b=bk["bc"], j=j: e.activation(out=bcs[:, 0, 0:HALO], in_=g.ps[b][:, 0:HALO], func=AF.Copy),
                 rd=[("ps", bk["bc"])], wr=[("bcs", 0)])
            s.op("dve", lambda e, b=bk["bx"], j=j: e.tensor_tensor(out=cxbuf[:, j, 0:2], in0=g.ps[b][:, 30:HALO], in1=bcs[:, 0, 30:HALO], op=ALU.mult),
                 rd=[("ps", bk["bx"]), ("bcs", 0)], wr=[("cxbuf", j)])

        def front(t):
            tc = slice(t * TW, (t + 1) * TW)
            if DBG.get("v") == 1:
                for q in range(4):
                    s.dma("sp", lambda e, t=t, q=q: e.dma_start(out=xt[:, q, :], in_=xin_t[t][:, q, :]), "m0xt", wr=["xt"])
            else:
                s.dma("sp", lambda e, t=t: e.dma_start(out=xt[:], in_=xin_t[t]), "m0xt", wr=["xt"])
            for c in range(KC):
                b = next_bank(g, "m0", PB)
                s.op("pe", [lambda e, c=c, b=b, q=q: e.transpose(g.ps[b][:, q * 128:(q + 1) * 128], xt[:, q, c * 128:(c + 1) * 128], g.ident[:])
                            for q in range(4)], rd=["xt", "ident"], wr=[("ps", b)])
                if DBG.get("v") == 5:
                    s.op("dve", lambda e, c=c, b=b, tc=tc: e.tensor_scalar(g.xres[:, c, tc], g.ps[b][:], ALPHA, None, ALU.mult),
                         rd=[("ps", b)], wr=[("xres", c, t)])
                else:
                    s.op("act", lambda e, c=c, b=b, tc=tc: e.activation(out=g.xres[:, c, tc], in_=g.ps[b][:], func=AF.Identity, scale=ALPHA),
                         rd=[("ps", b)], wr=[("xres", c, t)])
                if DBG.get("v", 3) == 3:
                    s.op("act", lambda e, c=c, b=b: e.activation(out=xbt[:, c, :], in_=g.ps[b][:], func=AF.Copy), rd=[("ps", b)], wr=[("xbt", c)])
                elif DBG.get("v") == 4:
                    s.op("dve", lambda e, c=c, b=b, tc=tc: e.tensor_scalar(xbt[:, c, :], g.xres[:, c, tc], 1.0 / ALPHA, None, ALU.mult), rd=[("xres", c, t)], wr=[("xbt", c)])
                elif DBG.get("v") != 2:
                    s.op("dve", lambda e, c=c, b=b: e.tensor_copy(out=xbt[:, c, :], in_=g.ps[b][:]), rd=[("ps", b)], wr=[("xbt", c)])

        def build_dg(j):
            par = j % 2
            s.op("dve", lambda e, j=j, par=par: e.tensor_tensor(out=dgv[par][:, 0:31, :], in0=g.ident[:].unsqueeze(1).to_broadcast([128, 31, 128]),
                                                                  in1=pvs(g, "a_dw", j * 31, 31).unsqueeze(2).to_broadcast([128, 31, 128]), op=ALU.mult),
                 rd=["ident", "pv"], wr=dg_alias[par])

        front(0)
        for t in range(NT):
            tc = slice(t * TW, (t + 1) * TW)
            if lvl >= 4:
                build_dg(0)
                build_dg(1)
            xbt_res = [("xbt", c) for c in range(KC)]
            if lvl < 4:
                continue
            for j in range(4):
                bk = {}
                for nm, strm in (("av", 0), ("ag", 1), ("bb", 2), ("bc", 3), ("bx", 4)):
                    m = strm * 4 + j
                    b = next_bank(g, "m0", PB)
                    bk[nm] = b
                    mm_group(g, g.ps[b][:], [(win[:, k, m * 128:(m + 1) * 128], xbt[:, k, :]) for k in range(KC)],
                             rd=winres + xbt_res, wr=[("ps", b)])
                s.op("act", lambda e, b=bk["ag"], j=j: e.activation(out=sig[:, 0, :], in_=g.ps[b][:], func=AF.Sigmoid),
                     rd=[("ps", bk["ag"])], wr=[("sig", 0)])
                s.op("dve", lambda e, b=bk["av"], j=j: e.tensor_tensor(out=abuf[:, j, 30:30 + TW], in0=g.ps[b][:], in1=sig[:, 0, :], op=ALU.mult),
                     rd=[("ps", bk["av"]), ("sig", 0)], wr=[("abuf", j)])
                s.op("act", lambda e, b=bk["bc"], j=j: e.activation(out=bcs[:, 0, :], in_=g.ps[b][:], func=AF.Copy),
                     rd=[("ps", bk["bc"])], wr=[("bcs", 0)])
                s.op("dve", lambda e, b=bk["bx"], j=j: e.tensor_tensor(out=cxbuf[:, j, 2:2 + TW], in0=g.ps[b][:], in1=bcs[:, 0, :], op=ALU.mult),
                     rd=[("ps", bk["bx"]), ("bcs", 0)], wr=[("cxbuf", j)])
                s.op("dve", lambda e, j=j: e.tensor_scalar(cb[:, j % 2, :], cxbuf[:, j, 0:TW], pvs(g, "b_dw", j * 3), None, ALU.mult),
                     rd=[("cxbuf", j), "pv"], wr=[("cb", j % 2)])
                for k in (1, 2):
                    s.op("dve", lambda e, j=j, k=k: e.scalar_tensor_tensor(out=cb[:, j % 2, :], in0=cxbuf[:, j, k:k + TW], scalar=pvs(g, "b_dw", j * 3 + k),
                                                                          in1=cb[:, j % 2, :], op0=ALU.mult, op1=ALU.add),
                         rd=[("cxbuf", j), ("cb", j % 2)], wr=[("cb", j % 2)])
                s.op("dve", lambda e, b=bk["bb"], j=j: e.tensor_tensor(out=mix[:, 4 + j, :], in0=g.ps[b][:], in1=cb[:, j % 2, :], op=ALU.mult),
                     rd=[("ps", bk["bb"]), ("cb", j % 2)], wr=[("mix", 4 + j)])
                s.op("dve", lambda e, j=j: e.tensor_copy(out=cxbuf[:, j, 0:2], in_=cxbuf[:, j, TW:TW + 2]), rd=[("cxbuf", j)], wr=[("cxbuf", j)])
            def conv_mm(j):
                b = next_bank(g, "m0", PB)
                mm_group(g, g.ps[b][:], [(dgv[j % 2][:, k, :], abuf[:, j, k:k + TW]) for k in range(31)],
                         rd=dg_alias[j % 2] + [("abuf", j)], wr=[("ps", b)])
                s.op("act", lambda e, j=j, b=b: e.activation(out=cv[:, j, :], in_=g.ps[b][:], func=AF.Identity, bias=pvs(g, "a_dw_b", j)),
                     rd=[("ps", b), "pv"], wr=[("cv", j)])
            conv_mm(0)
            build_dg(2)
            conv_mm(1)
            build_dg(3)
            conv_mm(2)
            conv_mm(3)
            for j in range(4):
                s.op("dve", lambda e, j=j: e.tensor_copy(out=abuf[:, j, 0:30], in_=abuf[:, j, TW:TW + 30]), rd=[("abuf", j)], wr=[("abuf", j)])
            if lvl < 5:
                continue
            layer_norm_tile(g, t, 4, lambda c: cv[:, c, :], lambda c: ("cv", c), g.onesA, "onesA",
                            lambda c: [(mix[:, c, :], ("mix", c), pvs(g, "a_ln_g", c), pvs(g, "a_ln_b", c))],
                            tmp, (6, 7), func=AF.Silu)
            if lvl < 6:
                continue
            mixres = [("mix", c) for c in range(KC)]
            for n in range(KC):
                b = next_bank(g, "m0", PB)
                mm_group(g, g.ps[b][:], [(wout[:, k, n * 128:(n + 1) * 128], mix[:, k, :]) for k in range(KC)],
                         rd=woutres + mixres, wr=[("ps", b)])
                s.op("dve", lambda e, b=b, n=n, tc=tc: e.tensor_tensor(out=g.xres[:, n, tc], in0=g.ps[b][:], in1=g.xres[:, n, tc], op=ALU.add),
                     rd=[("ps", b), ("xres", n, t)], wr=[("xres", n, t)])
            if lvl < 7:
                continue
            if t + 1 < NT:
                front(t + 1)
            layer_norm_tile(g, t, KC, lambda c, tc=tc: g.xres[:, c, tc], lambda c, t=t: ("xres", c, t), g.onesD, "onesD",
                            lambda c, tc=tc, t=t: [(g.xres[:, c, tc], ("xres", c, t), g.pvx[:, c:c + 1], g.pvx[:, 8 + c:9 + c])],
                            tmp, (6, 7))


GELU_C = 2.0 * 0.7978845608028654


def mixer1(g, st0, mode, src, it=0):
    nc, s = g.nc, g.s
    full = (mode == "full")
    with ExitStack() as st:
        win = st.enter_context(g.sbuf("m1_win" + mode, [128, KC, 2048], BF16))
        wg = st.enter_context(g.sbuf("m1_wg" + mode, [128, 2, 4, 2, 256], BF16))
        if full:
            wout = st.enter_context(g.sbuf("m1_wout", [128, KC, D], BF16))
            mix = st.enter_context(g.sbuf("m1_mix", [128, KC, TW], BF16))
            nb = 1
            gy = st.enter_context(g.sbuf("m1_gy", [128, nb, TW], F32))
            gt = st.enter_context(g.sbuf("m1_gt", [128, nb, TW], F32))
            tmp = alloc_ln_tmp(g, st, "m1", nb)
            if src == "dram":
                hpa = st.enter_context(g.sbuf("m1_hpa", [128, NCORES, 16], F32))
        if src == "dram":
            xt = st.enter_context(g.sbuf("m1_xt" + mode, [128, 2, D], F32))
        xbt = st.enter_context(g.sbuf("m1_xbt" + mode, [128, KC, TW], BF16))
        xhb = st.enter_context(g.sbuf("m1_xhb" + mode, [128, KC, HALO], BF16))
        xrb = st.enter_context(g.sbuf("m1_xrb" + mode, [128, 1, 3 + TW], F32))
        xrh = st.enter_context(g.sbuf("m1_xrh" + mode, [128, KC, 3], F32))
        xc = st.enter_context(g.sbuf("m1_xc" + mode, [128, KC, TW], F32))
        xcb = st.enter_context(g.sbuf("m1_xcb" + mode, [128, KC, TW], BF16))
        rr = st.enter_context(g.sbuf("m1_r" + mode, [128, TW], F32))
        ii = st.enter_context(g.sbuf("m1_i" + mode, [128, TW], F32))
        aa = st.enter_context(g.sbuf("m1_a" + mode, [128, TW], F32))
        mu = st.enter_context(g.sbuf("m1_mu" + mode, [128, TW], F32))
        bb = st.enter_context(g.sbuf("m1_b" + mode, [128, TW], F32))
        hl = st.enter_context(g.sbuf("m1_hl" + mode, [128, 2, TW], F32))
        if not full:
            zer = st.enter_context(g.sbuf("m1_zer" + mode, [128, TW], F32))
            s.op("pool", lambda e: e.memset(zer[:], 0.0), wr=["zer"])
        sm = st.enter_context(g.sbuf("m1_sm" + mode, [128, 64], F32))
        g.epsc = st.enter_context(g.sbuf("m1_eps" + mode, [128, 1], F32))
        s.op("pool", lambda e, ep=g.epsc: e.memset(ep[:], LN_EPS), wr=["eps"])
        hst = sm[:, 0:8]
        pst = sm[:, 8:16]
        c1 = sm[:, 16:24]
        c2 = sm[:, 24:32]
        ee = sm[:, 32:40]
        uu = sm[:, 40:48]
        um = sm[:, 48:56]
        pm_ = sm[:, 56:64]
        M = mode

        load_w_bf(g, win, "m1win" + M, g.o_w_in, 2048, "m1win" + M)
        winres = [("m1win" + M, i) for i in range(4)]
        for gi, wsrc in enumerate((g.o_wga, g.o_wgx)):
            for h in range(4):
                s.dma("pool", lambda e, gi=gi, h=h, wsrc=wsrc: e.dma_start(out=wg[:, gi, h, :, :], in_=wsrc[h].rearrange("(kc p) n -> p kc n", p=128)),
                      ("m1wg" + M, gi, h), wr=[("m1wg", gi, h)])
        if full:
            load_w_bf(g, wout, "m1wout", g.o_w_out, D, "m1wout")
            woutres = [("m1wout", i) for i in range(2)]

        lam = pvs(g, "lam")
        s.op("act", lambda e: e.activation(out=ee, in_=lam, func=AF.Exp, scale=-1.0), rd=["pv"], wr=["sm_e"])
        s.op("dve", lambda e: e.tensor_scalar(uu, ee, 1.0, None, ALU.add), rd=["sm_e"], wr=["sm_u"])
        s.op("dve", lambda e: e.tensor_scalar(um, uu, -1.0, 1e-30, ALU.add, ALU.max), rd=["sm_u"], wr=["sm_um"])
        s.op("dve", lambda e: e.reciprocal(out=um, in_=um), rd=["sm_um"], wr=["sm_um"])
        s.op("dve", lambda e: e.tensor_tensor(out=um, in0=um, in1=ee, op=ALU.mult), rd=["sm_um", "sm_e"], wr=["sm_um"])
        s.op("act", lambda e: e.activation(out=uu, in_=uu, func=AF.Ln), rd=["sm_u"], wr=["sm_u"])
        s.op("dve", lambda e: e.tensor_tensor(out=uu, in0=uu, in1=um, op=ALU.mult), rd=["sm_u", "sm_um"], wr=["sm_u"])
        s.op("dve", lambda e: e.tensor_scalar(c1, uu, -8.0, None, ALU.mult), rd=["sm_u"], wr=["sm_c"])
        s.op("dve", lambda e: e.tensor_scalar(c2, uu, -16.0, None, ALU.mult), rd=["sm_u"], wr=["sm_c"])

        if src == "sbuf":
            s.op("dve", lambda e: e.tensor_copy(out=hst, in_=g.keep[:, 0:8]), rd=["keep"], wr=["hst"])
        elif full:
            s.dma("sp", lambda e: e.dma_start(out=hpa[:], in_=g.hp_all_d), "m1hpa", wr=["hpa"])
            s.op("dve", lambda e: e.memset(hst, 0.0), wr=["hst"])
            for j in range(NCORES):
                s.op("dve", lambda e, j=j: e.tensor_scalar(pm_, hpa[:, j, 8:16], pvs(g, "mrank", j), pvs(g, "mrank_c", j), ALU.mult, ALU.add),
                     rd=["hpa", "pv"], wr=["sm_pm"])
                s.op("dve", lambda e: e.tensor_tensor(out=hst, in0=hst, in1=pm_, op=ALU.mult), rd=["hst", "sm_pm"], wr=["hst"])
                s.op("dve", lambda e, j=j: e.tensor_scalar(pm_, hpa[:, j, 0:8], pvs(g, "mrank", j), None, ALU.mult),
                     rd=["hpa", "pv"], wr=["sm_pm"])
                s.op("dve", lambda e: e.tensor_tensor(out=hst, in0=hst, in1=pm_, op=ALU.add), rd=["hst", "sm_pm"], wr=["hst"])
        else:
            s.op("dve", lambda e: e.memset(hst, 0.0), wr=["hst"])
            s.op("dve", lambda e: e.memset(pst, 1.0), wr=["pst"])

        PB = [0, 1, 2, 3, 4, 5]
        if src == "dram":
            xin_t = g.xin[HALO:HALO + T, :].rearrange("(t q p) d -> t p q d", q=4, p=128)
            xh = xt[0:HALO, 0, :]
            s.dma("sp", lambda e: e.dma_start(out=xh, in_=g.xin[0:HALO, :]), "m1xh" + M, wr=["xt"])
            for c in range(KC):
                b = next_bank(g, "m1", PB)
                s.op("pe", lambda e, c=c, b=b: e.transpose(g.ps[b][:, 0:HALO], xh[:, c * 128:(c + 1) * 128], g.ident[0:HALO, 0:HALO]),
                     rd=["xt", "ident"], wr=[("ps", b)])
                s.op("dve", lambda e, c=c, b=b: e.tensor_copy(out=xhb[:, c, :], in_=g.ps[b][:, 0:HALO]), rd=[("ps", b)], wr=[("xhb", c)])
        xhb_res = [("xhb", c) for c in range(KC)]
        if src == "sbuf":
            s.op("dve", lambda e: e.tensor_copy(out=xrh[:].rearrange("p c k -> p (c k)"), in_=g.keep[:, 8:32]), rd=["keep"],
                 wr=[("xrh", c) for c in range(KC)])
        for c in range(KC if src == "dram" else 0):
            b = next_bank(g, "m1", PB)
            m = 8 + c
            mm_group(g, g.ps[b][:, 0:HALO], [(win[:, k, m * 128:(m + 1) * 128], xhb[:, k, :]) for k in range(KC)],
                     rd=winres + xhb_res, wr=[("ps", b)])
            s.op("dve", lambda e, c=c, b=b: e.tensor_copy(out=xrh[:, c, :], in_=g.ps[b][:, HALO - 3:HALO]), rd=[("ps", b)], wr=[("xrh", c)])

        for t in range(NT):
            tc = slice(t * TW, (t + 1) * TW)
            if src == "dram":
                for h2 in range(2):
                    s.dma("sp", lambda e, t=t, h2=h2: e.dma_start(out=xt[:], in_=xin_t[t][:, 2 * h2:2 * h2 + 2, :]), "m1xt" + M, wr=["xt"])
                    for c in range(KC):
                        b = next_bank(g, "m1", PB)
                        s.op("pe", [lambda e, c=c, b=b, q=q: e.transpose(g.ps[b][:, q * 128:(q + 1) * 128], xt[:, q, c * 128:(c + 1) * 128], g.ident[:])
                                    for q in range(2)], rd=["xt", "ident"], wr=[("ps", b)])
                        s.op("act", lambda e, c=c, b=b, t=t, h2=h2: e.activation(out=g.xres[:, c, t * TW + h2 * 256:t * TW + h2 * 256 + 256], in_=g.ps[b][:, 0:256], func=AF.Identity, scale=ALPHA),
                             rd=[("ps", b)], wr=[("xres", c, t)])
                        s.op("act", lambda e, c=c, b=b, h2=h2: e.activation(out=xbt[:, c, h2 * 256:h2 * 256 + 256], in_=g.ps[b][:, 0:256], func=AF.Copy), rd=[("ps", b)], wr=[("xbt", c)])
            else:
                for c in range(KC):
                    s.op("act", lambda e, c=c, tc=tc: e.activation(out=xbt[:, c, :], in_=g.xres[:, c, tc], func=AF.Copy, scale=1.0 / ALPHA),
                         rd=[("xres", c, t)], wr=[("xbt", c)])
            xbt_res = [("xbt", c) for c in range(KC)]
            for c in range(KC):
                b = next_bank(g, "m1", PB)
                m = 8 + c
                par = 0
                mm_group(g, g.ps[b][:], [(win[:, k, m * 128:(m + 1) * 128], xbt[:, k, :]) for k in range(KC)],
                         rd=winres + xbt_res, wr=[("ps", b)])
                s.op("act", lambda e, b=b, par=par: e.activation(out=xrb[:, par, 3:3 + TW], in_=g.ps[b][:], func=AF.Copy),
                     rd=[("ps", b)], wr=[("xrb", par)])
                s.op("dve", lambda e, c=c, par=par: e.tensor_copy(out=xrb[:, par, 0:3], in_=xrh[:, c, :]), rd=[("xrh", c)], wr=[("xrb", par)])
                s.op("dve", lambda e, c=c, par=par: e.tensor_scalar(xc[:, c, :], xrb[:, par, 0:TW], pvs(g, "c_dw", c * 4), pvs(g, "c_dw_b", c), ALU.mult, ALU.add),
                     rd=[("xrb", par), "pv"], wr=[("xc", c)])
                for k in (1, 2, 3):
                    s.op("dve", lambda e, c=c, k=k, par=par: e.scalar_tensor_tensor(out=xc[:, c, :], in0=xrb[:, par, k:k + TW], scalar=pvs(g, "c_dw", c * 4 + k),
                                                                                   in1=xc[:, c, :], op0=ALU.mult, op1=ALU.add),
                         rd=[("xrb", par), ("xc", c)], wr=[("xc", c)])
                s.op("dve", lambda e, c=c, par=par: e.tensor_copy(out=xrh[:, c, :], in_=xrb[:, par, TW:TW + 3]), rd=[("xrb", par)], wr=[("xrh", c)])
                s.op("act", lambda e, c=c: e.activation(out=xcb[:, c, :], in_=xc[:, c, :], func=AF.Copy), rd=[("xc", c)], wr=[("xcb", c)])
            for c in range(KC):
                h, mm_ = c // 2, c % 2
                ba = next_bank(g, "m1", PB)
                bx = next_bank(g, "m1", PB)
                mm_group(g, g.ps[ba][:], [(wg[:, 0, h, k, mm_ * 128:(mm_ + 1) * 128], xcb[:, 2 * h + k, :]) for k in range(2)],
                         rd=[("m1wg", 0, h), ("xcb", 2 * h), ("xcb", 2 * h + 1)], wr=[("ps", ba)])
                mm_group(g, g.ps[bx][:], [(wg[:, 1, h, k, mm_ * 128:(mm_ + 1) * 128], xcb[:, 2 * h + k, :]) for k in range(2)],
                         rd=[("m1wg", 1, h), ("xcb", 2 * h), ("xcb", 2 * h + 1)], wr=[("ps", bx)])
                s.op("act", lambda e, c=c, ba=ba: e.activation(out=rr[:], in_=g.ps[ba][:], func=AF.Sigmoid, bias=pvs(g, "b_ga", c)),
                     rd=[("ps", ba), "pv"], wr=["m1r"])
                s.op("act", lambda e, c=c, bx=bx: e.activation(out=ii[:], in_=g.ps[bx][:], func=AF.Sigmoid, bias=pvs(g, "b_gx", c)),
                     rd=[("ps", bx), "pv"], wr=["m1i"])
                s.op("act", lambda e, c=c: e.activation(out=aa[:], in_=rr[:], func=AF.Exp, scale=c1[:, c:c + 1]), rd=["m1r", "sm_c"], wr=["m1a"])
                s.op("act", lambda e, c=c: e.activation(out=mu[:], in_=rr[:], func=AF.Exp, scale=c2[:, c:c + 1]), rd=["m1r", "sm_c"], wr=["m1mu"])
                s.op("act", lambda e: e.activation(out=mu[:], in_=mu[:], func=AF.Sqrt, bias=1.0, scale=-1.0), rd=["m1mu"], wr=["m1mu"])
                s.op("dve", lambda e, c=c: e.tensor_tensor(out=bb[:], in0=ii[:], in1=xc[:, c, :], op=ALU.mult), rd=["m1i", ("xc", c)], wr=["m1b"])
                s.op("dve", lambda e: e.tensor_tensor(out=bb[:], in0=bb[:], in1=mu[:], op=ALU.mult), rd=["m1b", "m1mu"], wr=["m1b"])
                s.op("dve", lambda e, c=c: e.tensor_tensor_scan(out=hl[:, c % 2, :], data0=aa[:], data1=bb[:], initial=hst[:, c:c + 1], op0=ALU.mult, op1=ALU.add),
                     rd=["m1a", "m1b", "hst"], wr=[("hl", c % 2)])
                s.op("dve", lambda e, c=c: e.tensor_copy(out=hst[:, c:c + 1], in_=hl[:, c % 2, TW - 1:TW]), rd=[("hl", c % 2)], wr=["hst"])
                if not full:
                    if src == "dram":
                        s.op("dve", lambda e, c=c: e.tensor_tensor_scan(out=bb[:], data0=aa[:], data1=zer[:], initial=pst[:, c:c + 1], op0=ALU.mult, op1=ALU.add),
                             rd=["m1a", "zer", "pst", "m1b"], wr=["m1b"])
                        s.op("dve", lambda e, c=c: e.tensor_copy(out=pst[:, c:c + 1], in_=bb[:, TW - 1:TW]), rd=["m1b"], wr=["pst"])
                else:
                    by = next_bank(g, "m1", PB)
                    mm_group(g, g.ps[by][:], [(win[:, k, c * 128:(c + 1) * 128], xbt[:, k, :]) for k in range(KC)],
                             rd=winres + xbt_res, wr=[("ps", by)])
                    p2 = c % nb
                    s.op("act", lambda e, by=by, p2=p2: e.activation(out=gy[:, p2, :], in_=g.ps[by][:], func=AF.Copy), rd=[("ps", by)], wr=[("gy", p2)])
                    s.op("act", lambda e, p2=p2: e.activation(out=gt[:, p2, :], in_=gy[:, p2, :], func=AF.Square), rd=[("gy", p2)], wr=[("gt", p2)])
                    s.op("dve", lambda e, p2=p2: e.tensor_scalar(gt[:, p2, :], gt[:, p2, :], 0.044715, 1.0, ALU.mult, ALU.add), rd=[("gt", p2)], wr=[("gt", p2)])
                    s.op("dve", lambda e, p2=p2: e.tensor_tensor(out=gt[:, p2, :], in0=gt[:, p2, :], in1=gy[:, p2, :], op=ALU.mult), rd=[("gt", p2), ("gy", p2)], wr=[("gt", p2)])
                    s.op("act", lambda e, p2=p2: e.activation(out=gt[:, p2, :], in_=gt[:, p2, :], func=AF.Sigmoid, scale=GELU_C), rd=[("gt", p2)], wr=[("gt", p2)])
                    s.op("dve", lambda e, p2=p2: e.tensor_tensor(out=gy[:, p2, :], in0=gy[:, p2, :], in1=gt[:, p2, :], op=ALU.mult), rd=[("gt", p2), ("gy", p2)], wr=[("gy", p2)])
                    s.op("dve", lambda e, c=c, p2=p2: e.tensor_tensor(out=mix[:, c, :], in0=gy[:, p2, :], in1=hl[:, c % 2, :], op=ALU.mult),
                         rd=[("gy", p2), ("hl", c % 2)], wr=[("mix", c)])
            if not full:
                continue
            mixres = [("mix", c) for c in range(KC)]
            for n in range(KC):
                b = next_bank(g, "m1", PB)
                mm_group(g, g.ps[b][:], [(wout[:, k, n * 128:(n + 1) * 128], mix[:, k, :]) for k in range(KC)],
                         rd=woutres + mixres, wr=[("ps", b)])
                s.op("dve", lambda e, b=b, n=n, tc=tc: e.tensor_tensor(out=g.xres[:, n, tc], in0=g.ps[b][:], in1=g.xres[:, n, tc], op=ALU.add),
                     rd=[("ps", b), ("xres", n, t)], wr=[("xres", n, t)])
            layer_norm_tile(g, t, KC, lambda c, tc=tc: g.xres[:, c, tc], lambda c, t=t: ("xres", c, t), g.onesD, "onesD",
                            lambda c, tc=tc, t=t: [(g.xres[:, c, tc], ("xres", c, t), g.pvx[:, 32 + c:33 + c], g.pvx[:, 40 + c:41 + c])],
                            tmp, (6, 7))
        if not full and src == "sbuf":
            mk = pvs(g, "cmask", it)
            s.op("dve", lambda e: e.tensor_scalar(g.keep[:, 0:8], hst, mk, None, ALU.mult), rd=["hst", "pv"], wr=["keep"])
            s.op("dve", lambda e: e.tensor_scalar(g.keep[:, 8:32], xrh[:].rearrange("p c k -> p (c k)"), mk, None, ALU.mult),
                 rd=[("xrh", c) for c in range(KC)] + ["pv"], wr=["keep"])
        if not full and src == "dram":
            s.dma("sp", lambda e: e.dma_start(out=g.hp_out, in_=sm[:, 0:16]), "hpout", rd=["hst", "pst"], wr=["hpdram"])
            s.wait_res("sp", ["hpdram"])

def moe(g, st0, layer, final):
    nc, s = g.nc, g.s
    with ExitStack() as st:
        xbf = st.enter_context(g.sbuf("moe_xbf%d" % layer, [128, KC, T], BF16))
        ring = st.enter_context(g.sbuf("moe_ring%d" % layer, [128, RING, KC, TW], BF16))
        hb = st.enter_context(g.sbuf("moe_h%d" % layer, [128, 2, KC, TW], BF16))
        gbs = st.enter_context(g.sbuf("moe_gb%d" % layer, [128, 2, TW], F32))
        s1 = st.enter_context(g.sbuf("moe_s1%d" % layer, [128, 2, TW], F32))
        s2 = st.enter_context(g.sbuf("moe_s2%d" % layer, [128, 2, TW], F32))
        G = st.enter_context(g.sbuf("moe_G%d" % layer, [128, 16, NE], F32))
        dg = st.enter_context(g.sbuf("moe_dg%d" % layer, [128, 2, 128], F32))
        wr_sb = st.enter_context(g.sbuf("moe_wr%d" % layer, [128, KC, NE], F32))
        rt = st.enter_context(g.sbuf("moe_rt%d" % layer, [128, 2, 96], F32))
        g.epsc = st.enter_context(g.sbuf("moe_eps%d" % layer, [128, 1], F32))
        tmp = alloc_ln_tmp(g, st, "moe%d" % layer)
        s.op("pool", lambda e, ep=g.epsc: e.memset(ep[:], LN_EPS), wr=["eps"])
        L = "L%d" % layer

        pieces = []
        for e_ in range(NE):
            for (wt, half) in ((g.w1, 0), (g.w3, 0), (g.w1, 1), (g.w3, 1), (g.w2, 0), (g.w2, 1)):
                pieces.append((wt[layer][e_].rearrange("(kc p) n -> p kc n", p=128), half))
        state = {"next": 0}

        def issue_piece():
            p = state["next"]
            if p >= len(pieces):
                return
            state["next"] = p + 1
            slot = p % RING
            v, half = pieces[p]
            s.dma("pool", lambda e, v=v, half=half, slot=slot: e.dma_start(out=ring[:, slot, :, :], in_=v[:, :, half * TW:(half + 1) * TW]),
                  (L + "ring", slot), wr=[("ring", slot)])

        for _ in range(RING):
            issue_piece()

        def cast_tile(t):
            tc = slice(t * TW, (t + 1) * TW)
            for c in range(KC):
                eng = "act"
                if eng == "act":
                    s.op("act", lambda e, c=c, tc=tc: e.activation(out=xbf[:, c, tc], in_=g.xres[:, c, tc], func=AF.Copy, scale=1.0 / ALPHA),
                         rd=[("xres", c, t)], wr=[("xbf", c, t)])
                else:
                    s.op("pool", lambda e, c=c, tc=tc: e.tensor_scalar(xbf[:, c, tc], g.xres[:, c, tc], 1.0 / ALPHA, None, ALU.mult),
                         rd=[("xres", c, t)], wr=[("xbf", c, t)])

        s.dma("sp", lambda e: e.dma_start(out=wr_sb[:], in_=g.w_router.rearrange("(kc p) n -> p kc n", p=128)), L + "wr", wr=["wr_sb"])
        RB = 7

        def route_tile(i):
            t = i // 4
            r_ = rt[:, i % 2, :]
            rk = ("rt", i % 2)
            mm_group(g, g.ps[RB][:, 0:NE], [(g.xres[:, c, i * 128:(i + 1) * 128], wr_sb[:, c, :]) for c in range(KC)],
                     rd=[("xres", c, t) for c in range(KC)] + ["wr_sb"], wr=[("ps", RB)])
            lg = r_[:, 0:16]
            ex = r_[:, 16:32]
            p6 = r_[:, 32:56]
            gs = r_[:, 56:60]
            goh = r_[:, 60:64]
            pm = r_[:, 64:80]
            top8 = r_[:, 80:88]
            mx = r_[:, 88:89]
            gm = r_[:, 89:90]
            den = r_[:, 90:91]
            selm = r_[:, 16:32]
            ex4 = ex.rearrange("p (a b) -> p a b", b=4)
            p64 = p6.rearrange("p (a b) -> p a b", b=6)
            pm4 = pm.rearrange("p (a b) -> p a b", b=4)
            ops = [
                lambda e: e.scalar_tensor_tensor(out=lg, in0=g.ps[RB][:, 0:NE], scalar=1.0 / ALPHA, in1=pvs(g, "b_router"), op0=ALU.mult, op1=ALU.add),
                lambda e: e.tensor_reduce(out=mx, in_=lg, axis=AX.X, op=ALU.max, negate=True),
            ]
            s.op("dve", ops, rd=[("ps", RB), "pv"], wr=[rk])
            s.op("act", lambda e: e.activation(out=ex, in_=lg, func=AF.Exp, bias=mx), rd=[rk], wr=[rk])
            ops = [
                lambda e: e.tensor_tensor(out=p64[:, :, 0:3], in0=ex4[:, :, 0:3], in1=ex4[:, :, 1:4], op=ALU.add),
                lambda e: e.tensor_tensor(out=p64[:, :, 3:5], in0=ex4[:, :, 0:2], in1=ex4[:, :, 2:4], op=ALU.add),
                lambda e: e.tensor_tensor(out=p64[:, :, 5:6], in0=ex4[:, :, 0:1], in1=ex4[:, :, 3:4], op=ALU.add),
                lambda e: e.tensor_reduce(out=gs, in_=p64, axis=AX.X, op=ALU.max),
                lambda e: e.tensor_reduce(out=gm, in_=gs, axis=AX.X, op=ALU.max),
                lambda e: e.tensor_scalar(goh, gs, gm, None, ALU.is_ge),
                lambda e: e.tensor_tensor(out=pm4, in0=ex4, in1=goh.unsqueeze(2).to_broadcast([128, 4, 4]), op=ALU.mult),
                lambda e: e.max(out=top8, in_=pm),
                lambda e: e.tensor_scalar(selm, pm, top8[:, 1:2], None, ALU.is_ge),
                lambda e: e.tensor_tensor(out=den, in0=top8[:, 0:1], in1=top8[:, 1:2], op=ALU.add),
                lambda e: e.reciprocal(out=den, in_=den),
                lambda e: e.scalar_tensor_tensor(out=G[:, i, :], in0=pm, scalar=den, in1=selm, op0=ALU.mult, op1=ALU.mult),
            ]
            s.op("dve", ops, rd=[rk], wr=[rk, ("G", i)])

        def prep(t):
            cast_tile(t)
            for i in range(4 * t, 4 * t + 4):
                route_tile(i)

        UB = [0, 1, 2, 3]
        YB = [4, 5]
        GBK = 6
        xbf_res = {t: [("xbf", c, t) for c in range(KC)] for t in range(NT)}

        def slot_of(e_, idx):
            return (e_ * 6 + idx) % RING

        def phaseA(e_, t):
            par = (e_ * NT + t) % 2
            tc = slice(t * TW, (t + 1) * TW)
            for q in range(4):
                i = t * 4 + q
                s.op("dve", lambda e, i=i, q=q: e.tensor_scalar(dg[:, q % 2, :], g.ident[:], G[:, i, e_:e_ + 1], None, ALU.mult),
                     rd=[("G", i), "ident"], wr=[("dg", q % 2)])
                s.op("pe", lambda e, q=q: e.matmul(g.ps[GBK][:, q * 128:(q + 1) * 128], lhsT=g.ones1[:], rhs=dg[:, q % 2, :], start=True, stop=True),
                     rd=[("dg", q % 2), "ones1"], wr=[("ps", GBK)])
            s.op("act", lambda e: e.activation(out=gbs[:, par, :], in_=g.ps[GBK][:], func=AF.Copy), rd=[("ps", GBK)], wr=[("gbs", par)])
            for m in range(KC):
                half, lc = m // 4, (m % 4) * 128
                sl1, sl3 = slot_of(e_, 2 * half), slot_of(e_, 2 * half + 1)
                b1 = next_bank(g, "moeU", UB)
                b3 = next_bank(g, "moeU", UB)
                mm_group(g, g.ps[b1][:], [(ring[:, sl1, k, lc:lc + 128], xbf[:, k, tc]) for k in range(KC)],
                         rd=[("ring", sl1)] + xbf_res[t], wr=[("ps", b1)])
                mm_group(g, g.ps[b3][:], [(ring[:, sl3, k, lc:lc + 128], xbf[:, k, tc]) for k in range(KC)],
                         rd=[("ring", sl3)] + xbf_res[t], wr=[("ps", b3)])
                s.op("act", lambda e, b1=b1, m=m: e.activation(out=s1[:, m % 2, :], in_=g.ps[b1][:], func=AF.Silu),
                     rd=[("ps", b1)], wr=[("s1", m % 2)])
                s.op("dve", lambda e, b3=b3, m=m: e.tensor_tensor(out=s2[:, m % 2, :], in0=g.ps[b3][:], in1=s1[:, m % 2, :], op=ALU.mult),
                     rd=[("ps", b3), ("s1", m % 2)], wr=[("s2", m % 2)])
                s.op("dve", lambda e, m=m: e.tensor_tensor(out=hb[:, par, m, :], in0=s2[:, m % 2, :], in1=gbs[:, par, :], op=ALU.mult),
                     rd=[("s2", m % 2), ("gbs", par)], wr=[("hb", par, m)])
                if t == NT - 1 and m % 4 == 3:
                    issue_piece()
                    issue_piece()

        def phaseB(e_, t):
            par = (e_ * NT + t) % 2
            tc = slice(t * TW, (t + 1) * TW)
            for n in range(KC):
                half, lc = n // 4, (n % 4) * 128
                sl2 = slot_of(e_, 4 + half)
                by = next_bank(g, "moeY", YB)
                mm_group(g, g.ps[by][:], [(ring[:, sl2, m, lc:lc + 128], hb[:, par, m, :]) for m in range(KC)],
                         rd=[("ring", sl2)] + [("hb", par, m) for m in range(KC)], wr=[("ps", by)])
                s.op("dve", lambda e, by=by, n=n: e.tensor_tensor(out=g.xres[:, n, tc], in0=g.ps[by][:], in1=g.xres[:, n, tc], op=ALU.add),
                     rd=[("ps", by), ("xres", n, t)], wr=[("xres", n, t)])
                if t == NT - 1 and n % 4 == 3:
                    issue_piece()

        def ln2_tile(t):
            tc = slice(t * TW, (t + 1) * TW)
            if final:
                of = lambda c, tc=tc, t=t: [(g.xres[:, c, tc], ("xres", c, t), pvs(g, "ln2_g%d" % layer, c), pvs(g, "ln2_b%d" % layer, c))]
            else:
                of = lambda c, tc=tc, t=t: [(g.xres[:, c, tc], ("xres", c, t), g.pvx[:, 16 + c:17 + c], g.pvx[:, 24 + c:25 + c])]
            layer_norm_tile(g, t, KC, lambda c, tc=tc: g.xres[:, c, tc], lambda c, t=t: ("xres", c, t), g.onesD, "onesD", of, tmp, (7, 6))

        steps = [(e_, t) for e_ in range(NE) for t in range(NT)]
        for i, (e_, t) in enumerate(steps):
            if e_ == 0:
                prep(t)
            phaseA(e_, t)
            if i > 0:
                pe_, pt_ = steps[i - 1]
                phaseB(pe_, pt_)
                if pe_ == NE - 1:
                    ln2_tile(pt_)
        phaseB(*steps[-1])
        ln2_tile(NT - 1)


def write_out(g, st0):
    nc, s = g.nc, g.s
    with ExitStack() as st:
        ot = st.enter_context(g.sbuf("ot", [128, 2, D], F32))
        outv = g.out.rearrange("(i p) d -> i p d", p=128)
        OB = [0, 1, 2, 3]
        for i in range(16):
            t = i // 4
            for hf in range(2):
                b = next_bank(g, "wo", OB)
                s.op("pe", [lambda e, b=b, q=q, hf=hf, i=i: e.transpose(g.ps[b][:, q * 128:(q + 1) * 128], g.xres[:, hf * 4 + q, i * 128:(i + 1) * 128], g.ident[:])
                            for q in range(4)], rd=[("xres", hf * 4 + q, t) for q in range(4)] + ["ident"], wr=[("ps", b)])
                if hf == 0:
                    s.op("act", lambda e, b=b, i=i: e.activation(out=ot[:, i % 2, 0:512], in_=g.ps[b][:], func=AF.Copy), rd=[("ps", b)], wr=[("ot", i % 2, 0)])
                else:
                    s.op("dve", lambda e, b=b, i=i: e.tensor_copy(out=ot[:, i % 2, 512:1024], in_=g.ps[b][:]), rd=[("ps", b)], wr=[("ot", i % 2, 1)])
            s.dma("sp", lambda e, i=i: e.dma_start(out=outv[i], in_=ot[:, i % 2, :]), ("out", i % 2), rd=[("ot", i % 2, 0), ("ot", i % 2, 1)],
                  wr=[("outdram", i)])
        s.wait_res("sp", [("outdram", i) for i in range(16)])


_PROG_CACHE = {}


def make_in_maps(inp, stage="full", hp_all=None):
    x = np.asarray(inp["x"], np.float32)
    maps = []
    shared = {
        "even_w_in": np.ascontiguousarray(inp["even_w_in"][0]),
        "even_w_out": np.ascontiguousarray(inp["even_w_out"][0]),
        "odd_w_in": np.ascontiguousarray(inp["odd_w_in"][0]),
        "odd_w_gate_a": np.ascontiguousarray(inp["odd_w_gate_a"][0]),
        "odd_w_gate_x": np.ascontiguousarray(inp["odd_w_gate_x"][0]),
        "odd_w_out": np.ascontiguousarray(inp["odd_w_out"][0]),
        "w_router": np.ascontiguousarray(inp["w_router"]),
    }
    for l in range(DEPTH):
        if (l == 0 and stage in ("full", "l0", "fused")) or (l == 1 and stage in ("full", "l1f", "fused")):
            for nm in ("moe_w1", "moe_w3", "moe_w2"):
                shared["%s_%d" % (nm, l)] = np.ascontiguousarray(inp[nm][l])
    if stage in ("l1f", "l1mix"):
        shared["hp_all"] = np.ascontiguousarray(hp_all, dtype=np.float32)
    for core in range(NCORES):
        b, q = core // 4, core % 4
        if stage == "fused":
            xin = np.zeros((4 * T + HALO, D), np.float32)
            n = (q + 1) * T
            xin[HALO + 4 * T - n:] = x[b, :n]
        else:
            xin = np.zeros((T + HALO, D), np.float32)
            xin[HALO:] = x[b, q * T:(q + 1) * T]
            if q > 0:
                xin[:HALO] = x[b, q * T - HALO:q * T]
        m = dict(shared)
        m["xin"] = xin
        m["pvec"] = pack_pvec(inp, core)
        maps.append(m)
    return maps


def _prog(stage):
    if stage not in _PROG_CACHE:
        _PROG_CACHE[stage] = build_program(stage)
    return _PROG_CACHE[stage]


def kernel_unfused(**inputs):
    inp = {k: np.asarray(v) for k, v in inputs.items()}
    cores = list(range(NCORES))
    r0 = run_bass_kernel_spmd(_prog("l0"), make_in_maps(inp, "l0"), core_ids=cores)
    x1 = np.stack([r0.results[c]["out"] for c in cores], axis=0).reshape(2, 4 * T, D)
    inp1 = dict(inp)
    inp1["x"] = x1
    r1 = run_bass_kernel_spmd(_prog("l1s"), make_in_maps(inp1, "l1s"), core_ids=cores)
    hp_all = np.ascontiguousarray(np.stack([r1.results[c]["hp"] for c in cores], axis=1))
    r2 = run_bass_kernel_spmd(_prog("l1f"), make_in_maps(inp1, "l1f", hp_all), core_ids=cores)
    out = np.stack([r2.results[c]["out"] for c in cores], axis=0)
    return out.reshape(2, 4 * T, D).astype(np.float32)


def kernel(**inputs):
    inp = {k: np.asarray(v) for k, v in inputs.items()}
    cores = list(range(NCORES))
    r = run_bass_kernel_spmd(_prog("fused"), make_in_maps(inp, "fused"), core_ids=cores)
    out = np.stack([r.results[c]["out"] for c in cores], axis=0)
    return out.reshape(2, 4 * T, D).astype(np.float32)
```

```python
from contextlib import ExitStack
import numpy as np
import concourse.bass as bass
import concourse.mybir as mybir
from concourse.bass_utils import run_bass_kernel_spmd

F32 = mybir.dt.float32
BF16 = mybir.dt.bfloat16
AF = mybir.ActivationFunctionType
ALU = mybir.AluOpType
AX = mybir.AxisListType

ENGS = ["pe", "act", "dve", "pool", "sp"]
NCORES = 8
T = 2048
NT = 4
TW = 512
D = 1024
KC = 8
HALO = 32
DEPTH = 2
ALPHA = (2.0 * DEPTH) ** 0.25
LN_EPS = 1e-5
NE = 16
RING = 7
DBG = {"lvl": 9}


class Sched:
    def __init__(self, nc):
        self.nc = nc
        self.prog = {e: [] for e in ENGS}
        self.cnt = {e: 0 for e in ENGS}
        self.seen = {e: {} for e in ENGS}
        self.lastw = {}
        self.reads = {}
        self.dma_cnt = {}
        self.same = {"pool", "dve", "act"}

    def _deps(self, rd, wr):
        deps = []
        for r in rd:
            t = self.lastw.get(r)
            if t is not None:
                deps.append(t)
        for w in wr:
            t = self.lastw.get(w)
            if t is not None:
                deps.append(t)
            deps.extend(self.reads.get(w, ()))
        return deps

    def _filter(self, eng, deps):
        need = {}
        for sk, v in deps:
            if sk == ("eng", eng) and eng not in self.same:
                continue
            if self.seen[eng].get(sk, 0) >= v:
                continue
            if need.get(sk, 0) < v:
                need[sk] = v
        for sk, v in need.items():
            self.seen[eng][sk] = v
        return list(need.items())

    def _commit(self, tick, rd, wr):
        for r in rd:
            self.reads.setdefault(r, []).append(tick)
        for w in wr:
            self.lastw[w] = tick
            self.reads[w] = []

    def op(self, eng, fns, rd=(), wr=()):
        if callable(fns):
            fns = [fns]
        if eng != "pe" and len(fns) > 1:
            for fn in fns:
                tick = self.op(eng, fn, rd, wr)
            return tick
        px = [("psx", r[1]) for r in rd if isinstance(r, tuple) and r[0] == "ps"]
        if px:
            wr = list(wr) + px
        waits = self._filter(eng, self._deps(rd, wr))
        self.cnt[eng] += 1
        sk = ("eng", eng)
        tick = (sk, self.cnt[eng])
        self.prog[eng].append((waits, fns, sk, 1))
        self._commit(tick, rd, wr)
        return tick

    def dma(self, q, fn, semkey, rd=(), wr=()):
        waits = self._filter(q, self._deps(rd, wr))
        sk = ("dma", semkey)
        self.dma_cnt[sk] = self.dma_cnt.get(sk, 0) + 16
        tick = (sk, self.dma_cnt[sk])
        self.prog[q].append((waits, [fn], sk, 16))
        self._commit(tick, rd, wr)
        return tick

    def wait_res(self, eng, res_list):
        deps = []
        for r in res_list:
            t = self.lastw.get(r)
            if t is not None:
                deps.append(t)
            deps.extend(self.reads.get(r, ()))
        waits = self._filter(eng, deps)
        self.prog[eng].append((waits, [], None, 0))

    def barrier(self):
        targets = [(("eng", e), self.cnt[e]) for e in ENGS if self.cnt[e] > 0] + list(self.dma_cnt.items())
        for e in ENGS:
            waits = self._filter(e, targets)
            self.prog[e].append((waits, [], None, 0))

    def emit(self, stack):
        nc = self.nc
        sems = {}
        keys = [("eng", e) for e in ENGS] + list(self.dma_cnt.keys())
        for i, k in enumerate(keys):
            sems[k] = stack.enter_context(nc.semaphore("s%d" % i))
        block = stack.enter_context(nc.Block())
        prog = self.prog

        def run(engname):
            def body(eng):
                for waits, fns, sk, inc in prog[engname]:
                    for wk, wv in waits:
                        eng.wait_ge(sems[wk], wv)
                    for i, fn in enumerate(fns):
                        ins = fn(eng)
                        if i == len(fns) - 1:
                            ins.then_inc(sems[sk], inc)
            return body

        block.tensor(run("pe"))
        block.scalar(run("act"))
        block.vector(run("dve"))
        block.gpsimd(run("pool"))
        block.sync(run("sp"))


def _pvec_layout():
    off = {}
    n = 0

    def add(name, w):
        nonlocal n
        off[name] = (n, w)
        n += w

    add("a_dw", 4 * 31)
    add("a_dw_b", 4)
    add("a_ln_g", 4)
    add("a_ln_b", 4)
    add("b_dw", 4 * 3)
    for l in range(DEPTH):
        add("ln1_g%d" % l, 8)
        add("ln1_b%d" % l, 8)
        add("ln2_g%d" % l, 8)
        add("ln2_b%d" % l, 8)
    add("c_dw", 8 * 4)
    add("c_dw_b", 8)
    add("b_ga", 8)
    add("b_gx", 8)
    add("lam", 8)
    add("b_router", 16)
    add("mrank", 8)
    add("mrank_c", 8)
    add("hsel", 8)
    add("cmask", 4)
    return off, n


PV_OFF, NV = _pvec_layout()


def _chunks(v, nchunk):
    return np.ascontiguousarray(v.reshape(nchunk, 128).T)


def pack_pvec(inp, core):
    pv = np.zeros((128, NV), np.float32)

    def put(name, arr):
        o, w = PV_OFF[name]
        pv[:, o:o + w] = arr.reshape(128, w)

    a_dw = inp["even_a_dw"][0]
    put("a_dw", np.ascontiguousarray(a_dw.reshape(31, 4, 128).transpose(2, 1, 0)))
    put("a_dw_b", _chunks(inp["even_a_dw_b"][0], 4))
    put("a_ln_g", _chunks(inp["even_a_ln_g"][0], 4))
    put("a_ln_b", _chunks(inp["even_a_ln_b"][0], 4))
    put("b_dw", np.ascontiguousarray(inp["even_b_dw"][0].reshape(3, 4, 128).transpose(2, 1, 0)))
    for l in range(DEPTH):
        put("ln1_g%d" % l, _chunks(inp["ln1_g"][l], 8))
        put("ln1_b%d" % l, _chunks(inp["ln1_b"][l], 8))
        put("ln2_g%d" % l, _chunks(inp["ln2_g"][l], 8))
        put("ln2_b%d" % l, _chunks(inp["ln2_b"][l], 8))
    put("c_dw", np.ascontiguousarray(inp["odd_c_dw"][0].reshape(4, 8, 128).transpose(2, 1, 0)))
    put("c_dw_b", _chunks(inp["odd_c_dw_b"][0], 8))
    put("b_ga", _chunks(inp["odd_b_gate_a"][0].reshape(-1), 8))
    put("b_gx", _chunks(inp["odd_b_gate_x"][0].reshape(-1), 8))
    put("lam", _chunks(inp["odd_lam"][0], 8))
    put("b_router", np.broadcast_to(inp["b_router"][None, :], (128, 16)))
    m = np.zeros(8, np.float32)
    b0 = (core // 4) * 4
    m[b0:core] = 1.0
    put("mrank", np.broadcast_to(m[None, :], (128, 8)))
    put("mrank_c", np.broadcast_to((1.0 - m)[None, :], (128, 8)))
    hs = np.zeros(8, np.float32)
    if core % 4 != 0:
        hs[core - 1] = 1.0
    put("hsel", np.broadcast_to(hs[None, :], (128, 8)))
    cm = np.array([1.0 if (core % 4) - 3 + j >= 0 else 0.0 for j in range(4)], np.float32)
    put("cmask", np.broadcast_to(cm[None, :], (128, 4)))
    return pv


class K:
    pass


def build_program(stage="full"):
    nc = bass.Bass("TRN2", target_bir_lowering=False)
    g = K()
    g.nc = nc
    g.uid = 0
    _orig_sbuf = nc.sbuf_tensor

    def _sbuf(name, shape, dtype, **kw):
        g.uid += 1
        return _orig_sbuf("%s_u%d" % (name, g.uid), shape, dtype, **kw)
    g.sbuf = _sbuf

    def din(name, shape):
        return nc.dram_tensor(name, list(shape), F32, kind="ExternalInput").ap()

    g.xin = din("xin", [(4 * T if stage == "fused" else T) + HALO, D])
    g.pvd = din("pvec", [128, NV])
    g.e_w_in = din("even_w_in", [D, 2560])
    g.e_w_out = din("even_w_out", [D, D])
    g.o_w_in = din("odd_w_in", [D, 2048])
    g.o_wga = din("odd_w_gate_a", [4, 256, 256])
    g.o_wgx = din("odd_w_gate_x", [4, 256, 256])
    g.o_w_out = din("odd_w_out", [D, D])
    g.w_router = din("w_router", [D, NE])
    g.w1, g.w3, g.w2 = {}, {}, {}
    for l in range(DEPTH):
        if (l == 0 and stage in ("full", "l0", "fused")) or (l == 1 and stage in ("full", "l1f", "fused")):
            g.w1[l] = din("moe_w1_%d" % l, [NE, D, D])
            g.w3[l] = din("moe_w3_%d" % l, [NE, D, D])
            g.w2[l] = din("moe_w2_%d" % l, [NE, D, D])
    if stage in ("l1f", "l1mix"):
        g.hp_all_d = din("hp_all", [128, NCORES, 16])
    if stage == "l1s":
        g.hp_out = nc.dram_tensor("hp", [128, 16], F32, kind="ExternalOutput").ap()
    else:
        g.out = nc.dram_tensor("out", [T, D], F32, kind="ExternalOutput").ap()
    with ExitStack() as st:
        s = Sched(nc)
        g.s = s
        g.xres = st.enter_context(nc.sbuf_tensor("xres", [128, KC, T], F32))
        g.pv = st.enter_context(nc.sbuf_tensor("pv", [128, NV], F32))
        g.ident = st.enter_context(nc.sbuf_tensor("ident", [128, 128], F32))
        g.ones1 = st.enter_context(nc.sbuf_tensor("ones1", [128, 128], F32))
        g.onesA = st.enter_context(nc.sbuf_tensor("onesA", [128, 128], F32))
        g.onesD = st.enter_context(nc.sbuf_tensor("onesD", [128, 128], F32))
        g.pvx = st.enter_context(nc.sbuf_tensor("pvx", [128, 64], F32))
        g.ps = [st.enter_context(nc.psum_tensor("ps%d" % i, [128, TW], F32)) for i in range(8)]
        g.bank_rr = {}

        setup_consts(g)
        if stage == "fused":
            g.keep = st.enter_context(nc.sbuf_tensor("keep", [128, 32], F32))
            s.op("dve", lambda e: e.memset(g.keep[:], 0.0), wr=["keep"])
            for j in range(4):
                mixer0(g, st, row0=j * T)
                s.barrier()
                moe(g, st, 0, final=False)
                s.barrier()
                if j < 3:
                    mixer1(g, st, "state", "sbuf", it=j)
                    s.barrier()
            mixer1(g, st, "full", "sbuf")
            s.barrier()
            moe(g, st, 1, final=True)
            s.barrier()
        if stage in ("full", "l0", "l0mix"):
            mixer0(g, st)
            s.barrier()
        if stage in ("full", "l0"):
            moe(g, st, 0, final=(stage == "l0"))
            s.barrier()
        if stage == "l1s":
            mixer1(g, st, "state", "dram")
        if stage in ("l1f", "l1mix"):
            mixer1(g, st, "full", "dram")
            s.barrier()
        if stage == "l1f":
            moe(g, st, 1, final=True)
            s.barrier()
        if stage != "l1s":
            write_out(g, st)
        s.emit(st)
    return nc


def pvs(g, name, i=None, w=1):
    o, n = PV_OFF[name]
    if i is None:
        return g.pv[:, o:o + n]
    return g.pv[:, o + i:o + i + w]


def next_bank(g, group, banks):
    i = g.bank_rr.get(group, 0)
    g.bank_rr[group] = i + 1
    b = banks[i % len(banks)]
    return b


def setup_consts(g):
    s = g.s
    s.dma("sp", lambda e: e.dma_start(out=g.pv[:], in_=g.pvd), "pv", wr=["pv"])
    s.op("pool", lambda e: e.memset(g.ones1[:], 1.0), wr=["ones1"])
    s.op("pool", lambda e: e.memset(g.onesA[:], 1.0 / 512.0), wr=["onesA"])
    s.op("pool", lambda e: e.memset(g.onesD[:], 1.0 / 1024.0), wr=["onesD"])
    s.op("pool", lambda e: e.affine_select(out=g.ident[:], in_=g.ones1[:], pattern=[[-1, 128]],
                                           compare_op=ALU.is_equal, fill=0.0, base=0, channel_multiplier=1),
         rd=["ones1"], wr=["ident"])
    for i, nm in enumerate(["ln1_g0", "ln1_b0", "ln2_g0", "ln2_b0", "ln1_g1", "ln1_b1"]):
        s.op("dve", lambda e, i=i, nm=nm: e.tensor_scalar(g.pvx[:, i * 8:(i + 1) * 8], pvs(g, nm), ALPHA, None, ALU.mult),
             rd=["pv"], wr=["pvx"])


def mm_group(g, out_ap, pairs, rd, wr):
    n = len(pairs)
    fns = []
    for i, (l, r) in enumerate(pairs):
        fns.append(lambda e, l=l, r=r, i=i: e.matmul(out_ap, lhsT=l, rhs=r, start=(i == 0), stop=(i == n - 1)))
    return g.s.op("pe", fns, rd=rd, wr=wr)


def layer_norm_tile(g, t, nch, src_fn, src_res_fn, ones, ones_res, out_fn, tmp, stat_banks, func=AF.Identity):
    s = g.s
    nb = tmp["nb"]
    epsap = g.epsc[:, 0:1]
    bm, bx = stat_banks
    bmk, bxk = ("ps", bm), ("ps", bx)
    pm, px = g.ps[bm], g.ps[bx]
    for c in range(nch):
        sq = tmp["sq"][:, c % nb, :]
        s.op("act", lambda e, c=c, sq=sq: e.activation(out=sq, in_=src_fn(c), func=AF.Square),
             rd=[src_res_fn(c)], wr=[("lnsq", c % nb)])
        s.op("pe", lambda e, c=c: e.matmul(pm[:], lhsT=ones[:], rhs=src_fn(c), start=(c == 0), stop=(c == nch - 1)),
             rd=[src_res_fn(c), ones_res], wr=[bmk])
        s.op("pe", lambda e, c=c, sq=sq: e.matmul(px[:], lhsT=ones[:], rhs=sq, start=(c == 0), stop=(c == nch - 1)),
             rd=[("lnsq", c % nb)], wr=[bxk])
    s.op("act", lambda e: e.activation(out=tmp["msq"][:], in_=pm[:], func=AF.Square), rd=[bmk], wr=["lnmsq"])
    s.op("dve", lambda e: e.tensor_tensor(out=tmp["var"][:], in0=px[:], in1=tmp["msq"][:], op=ALU.subtract),
         rd=[bxk, "lnmsq"], wr=["lnvar"])
    s.op("act", lambda e: e.activation(out=tmp["var"][:], in_=tmp["var"][:], func=AF.Sqrt, bias=epsap),
         rd=["lnvar", "eps"], wr=["lnvar"])
    s.op("dve", lambda e: e.reciprocal(out=tmp["rstd"][:], in_=tmp["var"][:]), rd=["lnvar"], wr=["lnrstd"])
    for c in range(nch):
        t1 = tmp["t1"][:, c % nb, :]
        t2 = tmp["t2"][:, c % nb, :]
        s.op("dve", lambda e, c=c, t1=t1: e.tensor_tensor(out=t1, in0=src_fn(c), in1=pm[:], op=ALU.subtract),
             rd=[src_res_fn(c), bmk], wr=[("lnt1", c % nb)])
        s.op("dve", lambda e, t1=t1, t2=t2: e.tensor_tensor(out=t2, in0=t1, in1=tmp["rstd"][:], op=ALU.mult),
             rd=[("lnt1", c % nb), "lnrstd"], wr=[("lnt2", c % nb)])
        for (oap, ores, gap, bap) in out_fn(c):
            s.op("act", lambda e, oap=oap, t2=t2, gap=gap, bap=bap: e.activation(out=oap, in_=t2, func=func, bias=bap, scale=gap),
                 rd=[("lnt2", c % nb), "pv", "pvx"], wr=[ores])


def alloc_ln_tmp(g, st, tag, nb=2):
    nc = g.nc
    tmp = {}
    tmp["nb"] = nb
    tmp["sq"] = st.enter_context(g.sbuf("lnsq" + tag, [128, nb, TW], F32))
    tmp["msq"] = st.enter_context(g.sbuf("lnmsq" + tag, [128, TW], F32))
    tmp["var"] = st.enter_context(g.sbuf("lnvar" + tag, [128, TW], F32))
    tmp["rstd"] = st.enter_context(g.sbuf("lnrstd" + tag, [128, TW], F32))
    tmp["t1"] = st.enter_context(g.sbuf("lnt1" + tag, [128, nb, TW], F32))
    tmp["t2"] = st.enter_context(g.sbuf("lnt2" + tag, [128, nb, TW], F32))
    return tmp


def load_w_bf(g, dst, dst_res, src2d, ncols, semkey, colchunk=512, c0=0):
    s = g.s
    v = src2d.rearrange("(kc p) n -> p kc n", p=128)
    for cc in range(0, ncols, colchunk):
        w = min(colchunk, ncols - cc)
        s.dma("pool", lambda e, cc=cc, w=w: e.dma_start(out=dst[:, :, cc:cc + w], in_=v[:, :, c0 + cc:c0 + cc + w]),
              (semkey, cc // colchunk), wr=[(dst_res, cc // colchunk)])


def mixer0(g, st0, row0=0):
    nc, s = g.nc, g.s
    with ExitStack() as st:
        win = st.enter_context(g.sbuf("m0_win", [128, KC, 2560], BF16))
        wout = st.enter_context(g.sbuf("m0_wout", [128, KC, D], BF16))
        xt = st.enter_context(g.sbuf("m0_xt", [128, 4, D], F32))
        xh = xt[0:HALO, 0, :]
        xbt = st.enter_context(g.sbuf("m0_xbt", [128, KC, TW], BF16))
        xhb = st.enter_context(g.sbuf("m0_xhb", [128, KC, HALO], BF16))
        abuf = st.enter_context(g.sbuf("m0_abuf", [128, 4, 30 + TW], BF16))
        cxbuf = st.enter_context(g.sbuf("m0_cxbuf", [128, 4, 2 + TW], F32))
        cv = st.enter_context(g.sbuf("m0_cv", [128, 4, TW], F32))
        sig = st.enter_context(g.sbuf("m0_sig", [128, 1, TW], F32))
        bcs = st.enter_context(g.sbuf("m0_bcs", [128, 1, TW], F32))
        cb = st.enter_context(g.sbuf("m0_cb", [128, 2, TW], F32))
        mix = st.enter_context(g.sbuf("m0_mix", [128, KC, TW], BF16))
        g.epsc = st.enter_context(g.sbuf("m0_eps", [128, 1], F32))
        scrA = st.enter_context(g.sbuf("m0_scrA", [128, 4 * TW], F32))
        scrB = st.enter_context(g.sbuf("m0_scrB", [128, 4 * TW], F32))
        tmp = {"nb": 2,
               "t1": scrA[:, 0:2 * TW].rearrange("p (a b) -> p a b", b=TW),
               "t2": scrA[:, 2 * TW:4 * TW].rearrange("p (a b) -> p a b", b=TW),
               "sq": scrB[:, 0:2 * TW].rearrange("p (a b) -> p a b", b=TW),
               "msq": scrB[:, 2 * TW:3 * TW],
               "var": scrB[:, 3 * TW:4 * TW]}
        tmp["rstd"] = st.enter_context(g.sbuf("m0_rstd", [128, TW], F32))
        dgv = [scrA[:].bitcast(BF16).rearrange("p (k m) -> p k m", m=128), scrB[:].bitcast(BF16).rearrange("p (k m) -> p k m", m=128)]
        dg_alias = [[("lnt1", 0), ("lnt1", 1), ("lnt2", 0), ("lnt2", 1)], [("lnsq", 0), ("lnsq", 1), "lnmsq", "lnvar"]]
        s.op("pool", lambda e, ep=g.epsc: e.memset(ep[:], LN_EPS), wr=["eps"])

        lvl = DBG["lvl"]
        if lvl < 1:
            return
        if lvl >= 2:
            load_w_bf(g, win, "m0win", g.e_w_in, 2560, "m0win")
            load_w_bf(g, wout, "m0wout", g.e_w_out, D, "m0wout")
        winres = [("m0win", i) for i in range(5)]
        woutres = [("m0wout", i) for i in range(2)]
        PB = [0, 1, 2, 3, 4, 5]
        xin_t = g.xin[row0 + HALO:row0 + HALO + T, :].rearrange("(t q p) d -> t p q d", q=4, p=128)

        if lvl >= 3:
            s.dma("sp", lambda e: e.dma_start(out=xh, in_=g.xin[row0:row0 + HALO, :]), "m0xh", wr=["xt"])
        for c in range(KC if lvl >= 3 else 0):
            b = next_bank(g, "m0", PB)
            s.op("pe", lambda e, c=c, b=b: e.transpose(g.ps[b][:, 0:HALO], xh[:, c * 128:(c + 1) * 128], g.ident[0:HALO, 0:HALO]),
                 rd=["xt", "ident"], wr=[("ps", b)])
            s.op("dve", lambda e, c=c, b=b: e.tensor_copy(out=xhb[:, c, :], in_=g.ps[b][:, 0:HALO]), rd=[("ps", b)], wr=[("xhb", c)])
        xhb_res = [("xhb", c) for c in range(KC)]
        for j in range(4 if lvl >= 3 else 0):
            bk = {}
            for nm, strm in (("av", 0), ("ag", 1), ("bc", 3), ("bx", 4)):
                m = strm * 4 + j
                b = next_bank(g, "m0", PB)
                bk[nm] = b
                mm_group(g, g.ps[b][:, 0:HALO], [(win[:, k, m * 128:(m + 1) * 128], xhb[:, k, :]) for k in range(KC)],
                         rd=winres + xhb_res, wr=[("ps", b)])
            s.op("act", lambda e, b=bk["ag"], j=j: e.activation(out=sig[:, 0, 0:HALO], in_=g.ps[b][:, 0:HALO], func=AF.Sigmoid),
                 rd=[("ps", bk["ag"])], wr=[("sig", 0)])
            s.op("dve", lambda e, b=bk["av"], j=j: e.tensor_tensor(out=abuf[:, j, 0:30], in0=g.ps[b][:, 2:HALO], in1=sig[:, 0, 2:HALO], op=ALU.mult),
                 rd=[("ps", bk["av"]), ("sig", 0)], wr=[("abuf", j)])
            s.op("act", lambda e, b=bk["bc"], j=j: e.activation(out=bcs[:, 0, 0:HALO], in_=g.ps[b][:, 0:HALO], func=AF.Copy),
                 rd=[("ps", bk["bc"])], wr=[("bcs", 0)])
            s.op("dve", lambda e, b=bk["bx"], j=j: e.tensor_tensor(out=cxbuf[:, j, 0:2], in0=g.ps[b][:, 30:HALO], in1=bcs[:, 0, 30:HALO], op=ALU.mult),
                 rd=[("ps", bk["bx"]), ("bcs", 0)], wr=[("cxbuf", j)])

        def front(t):
            tc = slice(t * TW, (t + 1) * TW)
            if DBG.get("v") == 1:
                for q in range(4):
                    s.dma("sp", lambda e, t=t, q=q: e.dma_start(out=xt[:, q, :], in_=xin_t[t][:, q, :]), "m0xt", wr=["xt"])
            else:
                s.dma("sp", lambda e, t=t: e.dma_start(out=xt[:], in_=xin_t[t]), "m0xt", wr=["xt"])
            for c in range(KC):
                b = next_bank(g, "m0", PB)
                s.op("pe", [lambda e, c=c, b=b, q=q: e.transpose(g.ps[b][:, q * 128:(q + 1) * 128], xt[:, q, c * 128:(c + 1) * 128], g.ident[:])
                            for q in range(4)], rd=["xt", "ident"], wr=[("ps", b)])
                if DBG.get("v") == 5:
                    s.op("dve", lambda e, c=c, b=b, tc=tc: e.tensor_scalar(g.xres[:, c, tc], g.ps[b][:], ALPHA, None, ALU.mult),
                         rd=[("ps", b)], wr=[("xres", c, t)])
                else:
                    s.op("act", lambda e, c=c, b=b, tc=tc: e.activation(out=g.xres[:, c, tc], in_=g.ps[b][:], func=AF.Identity, scale=ALPHA),
                         rd=[("ps", b)], wr=[("xres", c, t)])
                if DBG.get("v", 3) == 3:
                    s.op("act", lambda e, c=c, b=b: e.activation(out=xbt[:, c, :], in_=g.ps[b][:], func=AF.Copy), rd=[("ps", b)], wr=[("xbt", c)])
                elif DBG.get("v") == 4:
                    s.op("dve", lambda e, c=c, b=b, tc=tc: e.tensor_scalar(xbt[:, c, :], g.xres[:, c, tc], 1.0 / ALPHA, None, ALU.mult), rd=[("xres", c, t)], wr=[("xbt", c)])
                elif DBG.get("v") != 2:
                    s.op("dve", lambda e, c=c, b=b: e.tensor_copy(out=xbt[:, c, :], in_=g.ps[b][:]), rd=[("ps", b)], wr=[("xbt", c)])

        def build_dg(j):
            par = j % 2
            s.op("dve", lambda e, j=j, par=par: e.tensor_tensor(out=dgv[par][:, 0:31, :], in0=g.ident[:].unsqueeze(1).to_broadcast([128, 31, 128]),
                                                                  in1=pvs(g, "a_dw", j * 31, 31).unsqueeze(2).to_broadcast([128, 31, 128]), op=ALU.mult),
                 rd=["ident", "pv"], wr=dg_alias[par])

        front(0)
        for t in range(NT):
            tc = slice(t * TW, (t + 1) * TW)
            if lvl >= 4:
                build_dg(0)
                build_dg(1)
            xbt_res = [("xbt", c) for c in range(KC)]
            if lvl < 4:
                continue
            for j in range(4):
                bk = {}
                for nm, strm in (("av", 0), ("ag", 1), ("bb", 2), ("bc", 3), ("bx", 4)):
                    m = strm * 4 + j
                    b = next_bank(g, "m0", PB)
                    bk[nm] = b
                    mm_group(g, g.ps[b][:], [(win[:, k, m * 128:(m + 1) * 128], xbt[:, k, :]) for k in range(KC)],
                             rd=winres + xbt_res, wr=[("ps", b)])
                s.op("act", lambda e, b=bk["ag"], j=j: e.activation(out=sig[:, 0, :], in_=g.ps[b][:], func=AF.Sigmoid),
                     rd=[("ps", bk["ag"])], wr=[("sig", 0)])
                s.op("dve", lambda e, b=bk["av"], j=j: e.tensor_tensor(out=abuf[:, j, 30:30 + TW], in0=g.ps[b][:], in1=sig[:, 0, :], op=ALU.mult),
                     rd=[("ps", bk["av"]), ("sig", 0)], wr=[("abuf", j)])
                s.op("act", lambda e, b=bk["bc"], j=j: e.activation(out=bcs[:, 0, :], in_=g.ps[b][:], func=AF.Copy),
                     rd=[("ps", bk["bc"])], wr=[("bcs", 0)])
                s.op("dve", lambda e, b=bk["bx"], j=j: e.tensor_tensor(out=cxbuf[:, j, 2:2 + TW], in0=g.ps[b][:], in1=bcs[:, 0, :], op=ALU.mult),
                     rd=[("ps", bk["bx"]), ("bcs", 0)], wr=[("cxbuf", j)])
                s.op("dve", lambda e, j=j: e.tensor_scalar(cb[:, j % 2, :], cxbuf[:, j, 0:TW], pvs(g, "b_dw", j * 3), None, ALU.mult),
                     rd=[("cxbuf", j), "pv"], wr=[("cb", j % 2)])
                for k in (1, 2):
                    s.op("dve", lambda e, j=j, k=k: e.scalar_tensor_tensor(out=cb[:, j % 2, :], in0=cxbuf[:, j, k:k + TW], scalar=pvs(g, "b_dw", j * 3 + k),
                                                                          in1=cb[:, j % 2, :], op0=ALU.mult, op1=ALU.add),
                         rd=[("cxbuf", j), ("cb", j % 2)], wr=[("cb", j % 2)])
                s.op("dve", lambda e, b=bk["bb"], j=j: e.tensor_tensor(out=mix[:, 4 + j, :], in0=g.ps[b][:], in1=cb[:, j % 2, :], op=ALU.mult),
                     rd=[("ps", bk["bb"]), ("cb", j % 2)], wr=[("mix", 4 + j)])
                s.op("dve", lambda e, j=j: e.tensor_copy(out=cxbuf[:, j, 0:2], in_=cxbuf[:, j, TW:TW + 2]), rd=[("cxbuf", j)], wr=[("cxbuf", j)])
            def conv_mm(j):
                b = next_bank(g, "m0", PB)
                mm_group(g, g.ps[b][:], [(dgv[j % 2][:, k, :], abuf[:, j, k:k + TW]) for k in range(31)],
                         rd=dg_alias[j % 2] + [("abuf", j)], wr=[("ps", b)])
                s.op("act", lambda e, j=j, b=b: e.activation(out=cv[:, j, :], in_=g.ps[b][:], func=AF.Identity, bias=pvs(g, "a_dw_b", j)),
                     rd=[("ps", b), "pv"], wr=[("cv", j)])
            conv_mm(0)
            build_dg(2)
            conv_mm(1)
            build_dg(3)
            conv_mm(2)
            conv_mm(3)
            for j in range(4):
                s.op("dve", lambda e, j=j: e.tensor_copy(out=abuf[:, j, 0:30], in_=abuf[:, j, TW:TW + 30]), rd=[("abuf", j)], wr=[("abuf", j)])
            if lvl < 5:
                continue
            layer_norm_tile(g, t, 4, lambda c: cv[:, c, :], lambda c: ("cv", c), g.onesA, "onesA",
                            lambda c: [(mix[:, c, :], ("mix", c), pvs(g, "a_ln_g", c), pvs(g, "a_ln_b", c))],
                            tmp, (6, 7), func=AF.Silu)
            if lvl < 6:
                continue
            mixres = [("mix", c) for c in range(KC)]
            for n in range(KC):
                b = next_bank(g, "m0", PB)
                mm_group(g, g.ps[b][:], [(wout[:, k, n * 128:(n + 1) * 128], mix[:, k, :]) for k in range(KC)],
                         rd=woutres + mixres, wr=[("ps", b)])
                s.op("dve", lambda e, b=b, n=n, tc=tc: e.tensor_tensor(out=g.xres[:, n, tc], in0=g.ps[b][:], in1=g.xres[:, n, tc], op=ALU.add),
                     rd=[("ps", b), ("xres", n, t)], wr=[("xres", n, t)])
            if lvl < 7:
                continue
            if t + 1 < NT:
                front(t + 1)
            layer_norm_tile(g, t, KC, lambda c, tc=tc: g.xres[:, c, tc], lambda c, t=t: ("xres", c, t), g.onesD, "onesD",
                            lambda c, tc=tc, t=t: [(g.xres[:, c, tc], ("xres", c, t), g.pvx[:, c:c + 1], g.pvx[:, 8 + c:9 + c])],
                            tmp, (6, 7))


GELU_C = 2.0 * 0.7978845608028654


def mixer1(g, st0, mode, src, it=0):
    nc, s = g.nc, g.s
    full = (mode == "full")
    with ExitStack() as st:
        win = st.enter_context(g.sbuf("m1_win" + mode, [128, KC, 2048], BF16))
        wg = st.enter_context(g.sbuf("m1_wg" + mode, [128, 2, 4, 2, 256], BF16))
        if full:
            wout = st.enter_context(g.sbuf("m1_wout", [128, KC, D], BF16))
            mix = st.enter_context(g.sbuf("m1_mix", [128, KC, TW], BF16))
            nb = 1
            gy = st.enter_context(g.sbuf("m1_gy", [128, nb, TW], F32))
            gt = st.enter_context(g.sbuf("m1_gt", [128, nb, TW], F32))
            tmp = alloc_ln_tmp(g, st, "m1", nb)
            if src == "dram":
                hpa = st.enter_context(g.sbuf("m1_hpa", [128, NCORES, 16], F32))
        if src == "dram":
            xt = st.enter_context(g.sbuf("m1_xt" + mode, [128, 2, D], F32))
        xbt = st.enter_context(g.sbuf("m1_xbt" + mode, [128, KC, TW], BF16))
        xhb = st.enter_context(g.sbuf("m1_xhb" + mode, [128, KC, HALO], BF16))
        xrb = st.enter_context(g.sbuf("m1_xrb" + mode, [128, 1, 3 + TW], F32))
        xrh = st.enter_context(g.sbuf("m1_xrh" + mode, [128, KC, 3], F32))
        xc = st.enter_context(g.sbuf("m1_xc" + mode, [128, KC, TW], F32))
        xcb = st.enter_context(g.sbuf("m1_xcb" + mode, [128, KC, TW], BF16))
        NB4 = 1 if full else 4
        rr4 = st.enter_context(g.sbuf("m1_r" + mode, [128, NB4, TW], F32))
        ii4 = st.enter_context(g.sbuf("m1_i" + mode, [128, NB4, TW], F32))
        aa4 = st.enter_context(g.sbuf("m1_a" + mode, [128, NB4, TW], F32))
        mu4 = st.enter_context(g.sbuf("m1_mu" + mode, [128, NB4, TW], F32))
        rr, ii, aa, mu = rr4[:, 0, :], ii4[:, 0, :], aa4[:, 0, :], mu4[:, 0, :]
        bb = st.enter_context(g.sbuf("m1_b" + mode, [128, TW], F32))
        hl = st.enter_context(g.sbuf("m1_hl" + mode, [128, 2, TW], F32))
        if not full:
            zer = st.enter_context(g.sbuf("m1_zer" + mode, [128, TW], F32))
            s.op("pool", lambda e: e.memset(zer[:], 0.0), wr=["zer"])
        sm = st.enter_context(g.sbuf("m1_sm" + mode, [128, 64], F32))
        g.epsc = st.enter_context(g.sbuf("m1_eps" + mode, [128, 1], F32))
        s.op("pool", lambda e, ep=g.epsc: e.memset(ep[:], LN_EPS), wr=["eps"])
        hst = sm[:, 0:8]
        pst = sm[:, 8:16]
        c1 = sm[:, 16:24]
        c2 = sm[:, 24:32]
        ee = sm[:, 32:40]
        uu = sm[:, 40:48]
        um = sm[:, 48:56]
        pm_ = sm[:, 56:64]
        M = mode

        load_w_bf(g, win, "m1win" + M, g.o_w_in, 2048, "m1win" + M)
        winres = [("m1win" + M, i) for i in range(4)]
        for gi, wsrc in enumerate((g.o_wga, g.o_wgx)):
            for h in range(4):
                s.dma("pool", lambda e, gi=gi, h=h, wsrc=wsrc: e.dma_start(out=wg[:, gi, h, :, :], in_=wsrc[h].rearrange("(kc p) n -> p kc n", p=128)),
                      ("m1wg" + M, gi, h), wr=[("m1wg", gi, h)])
        if full:
            load_w_bf(g, wout, "m1wout", g.o_w_out, D, "m1wout")
            woutres = [("m1wout", i) for i in range(2)]

        lam = pvs(g, "lam")
        s.op("act", lambda e: e.activation(out=ee, in_=lam, func=AF.Exp, scale=-1.0), rd=["pv"], wr=["sm_e"])
        s.op("dve", lambda e: e.tensor_scalar(uu, ee, 1.0, None, ALU.add), rd=["sm_e"], wr=["sm_u"])
        s.op("dve", lambda e: e.tensor_scalar(um, uu, -1.0, 1e-30, ALU.add, ALU.max), rd=["sm_u"], wr=["sm_um"])
        s.op("dve", lambda e: e.reciprocal(out=um, in_=um), rd=["sm_um"], wr=["sm_um"])
        s.op("dve", lambda e: e.tensor_tensor(out=um, in0=um, in1=ee, op=ALU.mult), rd=["sm_um", "sm_e"], wr=["sm_um"])
        s.op("act", lambda e: e.activation(out=uu, in_=uu, func=AF.Ln), rd=["sm_u"], wr=["sm_u"])
        s.op("dve", lambda e: e.tensor_tensor(out=uu, in0=uu, in1=um, op=ALU.mult), rd=["sm_u", "sm_um"], wr=["sm_u"])
        s.op("dve", lambda e: e.tensor_scalar(c1, uu, -8.0, None, ALU.mult), rd=["sm_u"], wr=["sm_c"])
        s.op("dve", lambda e: e.tensor_scalar(c2, uu, -16.0, None, ALU.mult), rd=["sm_u"], wr=["sm_c"])

        if src == "sbuf":
            s.op("dve", lambda e: e.tensor_copy(out=hst, in_=g.keep[:, 0:8]), rd=["keep"], wr=["hst"])
        elif full:
            s.dma("sp", lambda e: e.dma_start(out=hpa[:], in_=g.hp_all_d), "m1hpa", wr=["hpa"])
            s.op("dve", lambda e: e.memset(hst, 0.0), wr=["hst"])
            for j in range(NCORES):
                s.op("dve", lambda e, j=j: e.tensor_scalar(pm_, hpa[:, j, 8:16], pvs(g, "mrank", j), pvs(g, "mrank_c", j), ALU.mult, ALU.add),
                     rd=["hpa", "pv"], wr=["sm_pm"])
                s.op("dve", lambda e: e.tensor_tensor(out=hst, in0=hst, in1=pm_, op=ALU.mult), rd=["hst", "sm_pm"], wr=["hst"])
                s.op("dve", lambda e, j=j: e.tensor_scalar(pm_, hpa[:, j, 0:8], pvs(g, "mrank", j), None, ALU.mult),
                     rd=["hpa", "pv"], wr=["sm_pm"])
                s.op("dve", lambda e: e.tensor_tensor(out=hst, in0=hst, in1=pm_, op=ALU.add), rd=["hst", "sm_pm"], wr=["hst"])
        else:
            s.op("dve", lambda e: e.memset(hst, 0.0), wr=["hst"])
            s.op("dve", lambda e: e.memset(pst, 1.0), wr=["pst"])

        PB = [0, 1, 2, 3, 4, 5]
        if src == "dram":
            xin_t = g.xin[HALO:HALO + T, :].rearrange("(t q p) d -> t p q d", q=4, p=128)
            xh = xt[0:HALO, 0, :]
            s.dma("sp", lambda e: e.dma_start(out=xh, in_=g.xin[0:HALO, :]), "m1xh" + M, wr=["xt"])
            for c in range(KC):
                b = next_bank(g, "m1", PB)
                s.op("pe", lambda e, c=c, b=b: e.transpose(g.ps[b][:, 0:HALO], xh[:, c * 128:(c + 1) * 128], g.ident[0:HALO, 0:HALO]),
                     rd=["xt", "ident"], wr=[("ps", b)])
                s.op("dve", lambda e, c=c, b=b: e.tensor_copy(out=xhb[:, c, :], in_=g.ps[b][:, 0:HALO]), rd=[("ps", b)], wr=[("xhb", c)])
        xhb_res = [("xhb", c) for c in range(KC)]
        if src == "sbuf":
            s.op("dve", lambda e: e.tensor_copy(out=xrh[:].rearrange("p c k -> p (c k)"), in_=g.keep[:, 8:32]), rd=["keep"],
                 wr=[("xrh", c) for c in range(KC)])
        for c in range(KC if src == "dram" else 0):
            b = next_bank(g, "m1", PB)
            m = 8 + c
            mm_group(g, g.ps[b][:, 0:HALO], [(win[:, k, m * 128:(m + 1) * 128], xhb[:, k, :]) for k in range(KC)],
                     rd=winres + xhb_res, wr=[("ps", b)])
            s.op("dve", lambda e, c=c, b=b: e.tensor_copy(out=xrh[:, c, :], in_=g.ps[b][:, HALO - 3:HALO]), rd=[("ps", b)], wr=[("xrh", c)])

        for t in range(NT):
            tc = slice(t * TW, (t + 1) * TW)
            if src == "dram":
                for h2 in range(2):
                    s.dma("sp", lambda e, t=t, h2=h2: e.dma_start(out=xt[:], in_=xin_t[t][:, 2 * h2:2 * h2 + 2, :]), "m1xt" + M, wr=["xt"])
                    for c in range(KC):
                        b = next_bank(g, "m1", PB)
                        s.op("pe", [lambda e, c=c, b=b, q=q: e.transpose(g.ps[b][:, q * 128:(q + 1) * 128], xt[:, q, c * 128:(c + 1) * 128], g.ident[:])
                                    for q in range(2)], rd=["xt", "ident"], wr=[("ps", b)])
                        s.op("act", lambda e, c=c, b=b, t=t, h2=h2: e.activation(out=g.xres[:, c, t * TW + h2 * 256:t * TW + h2 * 256 + 256], in_=g.ps[b][:, 0:256], func=AF.Identity, scale=ALPHA),
                             rd=[("ps", b)], wr=[("xres", c, t)])
                        s.op("act", lambda e, c=c, b=b, h2=h2: e.activation(out=xbt[:, c, h2 * 256:h2 * 256 + 256], in_=g.ps[b][:, 0:256], func=AF.Copy), rd=[("ps", b)], wr=[("xbt", c)])
            else:
                for c in range(KC):
                    s.op("act", lambda e, c=c, tc=tc: e.activation(out=xbt[:, c, :], in_=g.xres[:, c, tc], func=AF.Copy, scale=1.0 / ALPHA),
                         rd=[("xres", c, t)], wr=[("xbt", c)])
            xbt_res = [("xbt", c) for c in range(KC)]
            for c in range(KC):
                b = next_bank(g, "m1", PB)
                m = 8 + c
                par = 0
                mm_group(g, g.ps[b][:], [(win[:, k, m * 128:(m + 1) * 128], xbt[:, k, :]) for k in range(KC)],
                         rd=winres + xbt_res, wr=[("ps", b)])
                s.op("act", lambda e, b=b, par=par: e.activation(out=xrb[:, par, 3:3 + TW], in_=g.ps[b][:], func=AF.Copy),
                     rd=[("ps", b)], wr=[("xrb", par)])
                s.op("dve", lambda e, c=c, par=par: e.tensor_copy(out=xrb[:, par, 0:3], in_=xrh[:, c, :]), rd=[("xrh", c)], wr=[("xrb", par)])
                s.op("dve", lambda e, c=c, par=par: e.tensor_scalar(xc[:, c, :], xrb[:, par, 0:TW], pvs(g, "c_dw", c * 4), pvs(g, "c_dw_b", c), ALU.mult, ALU.add),
                     rd=[("xrb", par), "pv"], wr=[("xc", c)])
                for k in (1, 2, 3):
                    s.op("dve", lambda e, c=c, k=k, par=par: e.scalar_tensor_tensor(out=xc[:, c, :], in0=xrb[:, par, k:k + TW], scalar=pvs(g, "c_dw", c * 4 + k),
                                                                                   in1=xc[:, c, :], op0=ALU.mult, op1=ALU.add),
                         rd=[("xrb", par), ("xc", c)], wr=[("xc", c)])
                s.op("dve", lambda e, c=c, par=par: e.tensor_copy(out=xrh[:, c, :], in_=xrb[:, par, TW:TW + 3]), rd=[("xrb", par)], wr=[("xrh", c)])
                s.op("act", lambda e, c=c: e.activation(out=xcb[:, c, :], in_=xc[:, c, :], func=AF.Copy), rd=[("xc", c)], wr=[("xcb", c)])
            for c in (range(KC) if full else ()):
                h, mm_ = c // 2, c % 2
                ba = next_bank(g, "m1", PB)
                bx = next_bank(g, "m1", PB)
                mm_group(g, g.ps[ba][:], [(wg[:, 0, h, k, mm_ * 128:(mm_ + 1) * 128], xcb[:, 2 * h + k, :]) for k in range(2)],
                         rd=[("m1wg", 0, h), ("xcb", 2 * h), ("xcb", 2 * h + 1)], wr=[("ps", ba)])
                mm_group(g, g.ps[bx][:], [(wg[:, 1, h, k, mm_ * 128:(mm_ + 1) * 128], xcb[:, 2 * h + k, :]) for k in range(2)],
                         rd=[("m1wg", 1, h), ("xcb", 2 * h), ("xcb", 2 * h + 1)], wr=[("ps", bx)])
                s.op("act", lambda e, c=c, ba=ba: e.activation(out=rr, in_=g.ps[ba][:], func=AF.Sigmoid, bias=pvs(g, "b_ga", c)),
                     rd=[("ps", ba), "pv"], wr=["m1r"])
                s.op("act", lambda e, c=c, bx=bx: e.activation(out=ii, in_=g.ps[bx][:], func=AF.Sigmoid, bias=pvs(g, "b_gx", c)),
                     rd=[("ps", bx), "pv"], wr=["m1i"])
                s.op("act", lambda e, c=c: e.activation(out=aa, in_=rr, func=AF.Exp, scale=c1[:, c:c + 1]), rd=["m1r", "sm_c"], wr=["m1a"])
                s.op("act", lambda e, c=c: e.activation(out=mu, in_=rr, func=AF.Exp, scale=c2[:, c:c + 1]), rd=["m1r", "sm_c"], wr=["m1mu"])
                s.op("act", lambda e: e.activation(out=mu, in_=mu, func=AF.Sqrt, bias=1.0, scale=-1.0), rd=["m1mu"], wr=["m1mu"])
                s.op("dve", lambda e, c=c: e.tensor_tensor(out=bb[:], in0=ii, in1=xc[:, c, :], op=ALU.mult), rd=["m1i", ("xc", c)], wr=["m1b"])
                s.op("dve", lambda e: e.tensor_tensor(out=bb[:], in0=bb[:], in1=mu, op=ALU.mult), rd=["m1b", "m1mu"], wr=["m1b"])
                s.op("dve", lambda e, c=c: e.tensor_tensor_scan(out=hl[:, c % 2, :], data0=aa, data1=bb[:], initial=hst[:, c:c + 1], op0=ALU.mult, op1=ALU.add),
                     rd=["m1a", "m1b", "hst"], wr=[("hl", c % 2)])
                s.op("dve", lambda e, c=c: e.tensor_copy(out=hst[:, c:c + 1], in_=hl[:, c % 2, TW - 1:TW]), rd=[("hl", c % 2)], wr=["hst"])
                if not full:
                    if src == "dram":
                        s.op("dve", lambda e, c=c: e.tensor_tensor_scan(out=bb[:], data0=aa, data1=zer[:], initial=pst[:, c:c + 1], op0=ALU.mult, op1=ALU.add),
                             rd=["m1a", "zer", "pst", "m1b"], wr=["m1b"])
                        s.op("dve", lambda e, c=c: e.tensor_copy(out=pst[:, c:c + 1], in_=bb[:, TW - 1:TW]), rd=["m1b"], wr=["pst"])
                else:
                    by = next_bank(g, "m1", PB)
                    mm_group(g, g.ps[by][:], [(win[:, k, c * 128:(c + 1) * 128], xbt[:, k, :]) for k in range(KC)],
                             rd=winres + xbt_res, wr=[("ps", by)])
                    p2 = c % nb
                    s.op("act", lambda e, by=by, p2=p2: e.activation(out=gy[:, p2, :], in_=g.ps[by][:], func=AF.Copy), rd=[("ps", by)], wr=[("gy", p2)])
                    s.op("act", lambda e, p2=p2: e.activation(out=gt[:, p2, :], in_=gy[:, p2, :], func=AF.Square), rd=[("gy", p2)], wr=[("gt", p2)])
                    s.op("dve", lambda e, p2=p2: e.tensor_scalar(gt[:, p2, :], gt[:, p2, :], 0.044715, 1.0, ALU.mult, ALU.add), rd=[("gt", p2)], wr=[("gt", p2)])
                    s.op("dve", lambda e, p2=p2: e.tensor_tensor(out=gt[:, p2, :], in0=gt[:, p2, :], in1=gy[:, p2, :], op=ALU.mult), rd=[("gt", p2), ("gy", p2)], wr=[("gt", p2)])
                    s.op("act", lambda e, p2=p2: e.activation(out=gt[:, p2, :], in_=gt[:, p2, :], func=AF.Sigmoid, scale=GELU_C), rd=[("gt", p2)], wr=[("gt", p2)])
                    s.op("dve", lambda e, p2=p2: e.tensor_tensor(out=gy[:, p2, :], in0=gy[:, p2, :], in1=gt[:, p2, :], op=ALU.mult), rd=[("gt", p2), ("gy", p2)], wr=[("gy", p2)])
                    s.op("dve", lambda e, c=c, p2=p2: e.tensor_tensor(out=mix[:, c, :], in0=gy[:, p2, :], in1=hl[:, c % 2, :], op=ALU.mult),
                         rd=[("gy", p2), ("hl", c % 2)], wr=[("mix", c)])
            for hb in (range(2) if not full else ()):
                cs = list(range(4 * hb, 4 * hb + 4))
                for c in cs:
                    h, mm_, q4 = c // 2, c % 2, c % 4
                    ba = next_bank(g, "m1", PB)
                    bx = next_bank(g, "m1", PB)
                    mm_group(g, g.ps[ba][:], [(wg[:, 0, h, k, mm_ * 128:(mm_ + 1) * 128], xcb[:, 2 * h + k, :]) for k in range(2)],
                             rd=[("m1wg", 0, h), ("xcb", 2 * h), ("xcb", 2 * h + 1)], wr=[("ps", ba)])
                    mm_group(g, g.ps[bx][:], [(wg[:, 1, h, k, mm_ * 128:(mm_ + 1) * 128], xcb[:, 2 * h + k, :]) for k in range(2)],
                             rd=[("m1wg", 1, h), ("xcb", 2 * h), ("xcb", 2 * h + 1)], wr=[("ps", bx)])
                    s.op("act", lambda e, c=c, ba=ba, q4=q4: e.activation(out=rr4[:, q4, :], in_=g.ps[ba][:], func=AF.Sigmoid, bias=pvs(g, "b_ga", c)),
                         rd=[("ps", ba), "pv"], wr=[("m1r", q4)])
                    s.op("act", lambda e, c=c, bx=bx, q4=q4: e.activation(out=ii4[:, q4, :], in_=g.ps[bx][:], func=AF.Sigmoid, bias=pvs(g, "b_gx", c)),
                         rd=[("ps", bx), "pv"], wr=[("m1i", q4)])
                for c in cs:
                    q4 = c % 4
                    s.op("act", lambda e, c=c, q4=q4: e.activation(out=aa4[:, q4, :], in_=rr4[:, q4, :], func=AF.Exp, scale=c1[:, c:c + 1]),
                         rd=[("m1r", q4), "sm_c"], wr=[("m1a", q4)])
                    s.op("act", lambda e, c=c, q4=q4: e.activation(out=mu4[:, q4, :], in_=rr4[:, q4, :], func=AF.Exp, scale=c2[:, c:c + 1]),
                         rd=[("m1r", q4), "sm_c"], wr=[("m1mu", q4)])
                for c in cs:
                    q4 = c % 4
                    s.op("act", lambda e, q4=q4: e.activation(out=mu4[:, q4, :], in_=mu4[:, q4, :], func=AF.Sqrt, bias=1.0, scale=-1.0),
                         rd=[("m1mu", q4)], wr=[("m1mu", q4)])
                for c in cs:
                    q4 = c % 4
                    s.op("dve", lambda e, c=c, q4=q4: e.tensor_tensor(out=bb[:], in0=ii4[:, q4, :], in1=xc[:, c, :], op=ALU.mult), rd=[("m1i", q4), ("xc", c)], wr=["m1b"])
                    s.op("dve", lambda e, q4=q4: e.tensor_tensor(out=bb[:], in0=bb[:], in1=mu4[:, q4, :], op=ALU.mult), rd=["m1b", ("m1mu", q4)], wr=["m1b"])
                    s.op("dve", lambda e, c=c, q4=q4: e.tensor_tensor_scan(out=hl[:, c % 2, :], data0=aa4[:, q4, :], data1=bb[:], initial=hst[:, c:c + 1], op0=ALU.mult, op1=ALU.add),
                         rd=[("m1a", q4), "m1b", "hst"], wr=[("hl", c % 2)])
                    s.op("dve", lambda e, c=c: e.tensor_copy(out=hst[:, c:c + 1], in_=hl[:, c % 2, TW - 1:TW]), rd=[("hl", c % 2)], wr=["hst"])
                    if src == "dram":
                        s.op("dve", lambda e, c=c, q4=q4: e.tensor_tensor_scan(out=bb[:], data0=aa4[:, q4, :], data1=zer[:], initial=pst[:, c:c + 1], op0=ALU.mult, op1=ALU.add),
                             rd=[("m1a", q4), "zer", "pst", "m1b"], wr=["m1b"])
                        s.op("dve", lambda e, c=c: e.tensor_copy(out=pst[:, c:c + 1], in_=bb[:, TW - 1:TW]), rd=["m1b"], wr=["pst"])
            if not full:
                continue
            mixres = [("mix", c) for c in range(KC)]
            for n in range(KC):
                b = next_bank(g, "m1", PB)
                mm_group(g, g.ps[b][:], [(wout[:, k, n * 128:(n + 1) * 128], mix[:, k, :]) for k in range(KC)],
                         rd=woutres + mixres, wr=[("ps", b)])
                s.op("dve", lambda e, b=b, n=n, tc=tc: e.tensor_tensor(out=g.xres[:, n, tc], in0=g.ps[b][:], in1=g.xres[:, n, tc], op=ALU.add),
                     rd=[("ps", b), ("xres", n, t)], wr=[("xres", n, t)])
            layer_norm_tile(g, t, KC, lambda c, tc=tc: g.xres[:, c, tc], lambda c, t=t: ("xres", c, t), g.onesD, "onesD",
                            lambda c, tc=tc, t=t: [(g.xres[:, c, tc], ("xres", c, t), g.pvx[:, 32 + c:33 + c], g.pvx[:, 40 + c:41 + c])],
                            tmp, (6, 7))
        if not full and src == "sbuf":
            mk = pvs(g, "cmask", it)
            s.op("dve", lambda e: e.tensor_scalar(g.keep[:, 0:8], hst, mk, None, ALU.mult), rd=["hst", "pv"], wr=["keep"])
            s.op("dve", lambda e: e.tensor_scalar(g.keep[:, 8:32], xrh[:].rearrange("p c k -> p (c k)"), mk, None, ALU.mult),
                 rd=[("xrh", c) for c in range(KC)] + ["pv"], wr=["keep"])
        if not full and src == "dram":
            s.dma("sp", lambda e: e.dma_start(out=g.hp_out, in_=sm[:, 0:16]), "hpout", rd=["hst", "pst"], wr=["hpdram"])
            s.wait_res("sp", ["hpdram"])

def moe(g, st0, layer, final):
    nc, s = g.nc, g.s
    with ExitStack() as st:
        xbf = st.enter_context(g.sbuf("moe_xbf%d" % layer, [128, KC, T], BF16))
        ring = st.enter_context(g.sbuf("moe_ring%d" % layer, [128, RING, KC, TW], BF16))
        hb = st.enter_context(g.sbuf("moe_h%d" % layer, [128, 2, KC, TW], BF16))
        gbs = st.enter_context(g.sbuf("moe_gb%d" % layer, [128, 2, TW], F32))
        s1 = st.enter_context(g.sbuf("moe_s1%d" % layer, [128, 2, TW], F32))
        s2 = st.enter_context(g.sbuf("moe_s2%d" % layer, [128, 2, TW], F32))
        G = st.enter_context(g.sbuf("moe_G%d" % layer, [128, 16, NE], F32))
        dg = st.enter_context(g.sbuf("moe_dg%d" % layer, [128, 2, 128], F32))
        wr_sb = st.enter_context(g.sbuf("moe_wr%d" % layer, [128, KC, NE], F32))
        rt = st.enter_context(g.sbuf("moe_rt%d" % layer, [128, 2, 96], F32))
        g.epsc = st.enter_context(g.sbuf("moe_eps%d" % layer, [128, 1], F32))
        tmp = alloc_ln_tmp(g, st, "moe%d" % layer)
        s.op("pool", lambda e, ep=g.epsc: e.memset(ep[:], LN_EPS), wr=["eps"])
        L = "L%d" % layer

        pieces = []
        for e_ in range(NE):
            for (wt, half) in ((g.w1, 0), (g.w3, 0), (g.w1, 1), (g.w3, 1), (g.w2, 0), (g.w2, 1)):
                pieces.append((wt[layer][e_].rearrange("(kc p) n -> p kc n", p=128), half))
        state = {"next": 0}

        def issue_piece():
            p = state["next"]
            if p >= len(pieces):
                return
            state["next"] = p + 1
            slot = p % RING
            v, half = pieces[p]
            s.dma("pool", lambda e, v=v, half=half, slot=slot: e.dma_start(out=ring[:, slot, :, :], in_=v[:, :, half * TW:(half + 1) * TW]),
                  (L + "ring", slot), wr=[("ring", slot)])

        for _ in range(RING):
            issue_piece()

        def cast_tile(t):
            tc = slice(t * TW, (t + 1) * TW)
            for c in range(KC):
                eng = "act"
                if eng == "act":
                    s.op("act", lambda e, c=c, tc=tc: e.activation(out=xbf[:, c, tc], in_=g.xres[:, c, tc], func=AF.Copy, scale=1.0 / ALPHA),
                         rd=[("xres", c, t)], wr=[("xbf", c, t)])
                else:
                    s.op("pool", lambda e, c=c, tc=tc: e.tensor_scalar(xbf[:, c, tc], g.xres[:, c, tc], 1.0 / ALPHA, None, ALU.mult),
                         rd=[("xres", c, t)], wr=[("xbf", c, t)])

        s.dma("sp", lambda e: e.dma_start(out=wr_sb[:], in_=g.w_router.rearrange("(kc p) n -> p kc n", p=128)), L + "wr", wr=["wr_sb"])
        RB = 7

        def route_tile(i):
            t = i // 4
            r_ = rt[:, i % 2, :]
            rk = ("rt", i % 2)
            mm_group(g, g.ps[RB][:, 0:NE], [(g.xres[:, c, i * 128:(i + 1) * 128], wr_sb[:, c, :]) for c in range(KC)],
                     rd=[("xres", c, t) for c in range(KC)] + ["wr_sb"], wr=[("ps", RB)])
            lg = r_[:, 0:16]
            ex = r_[:, 16:32]
            p6 = r_[:, 32:56]
            gs = r_[:, 56:60]
            goh = r_[:, 60:64]
            pm = r_[:, 64:80]
            top8 = r_[:, 80:88]
            mx = r_[:, 88:89]
            gm = r_[:, 89:90]
            den = r_[:, 90:91]
            selm = r_[:, 16:32]
            ex4 = ex.rearrange("p (a b) -> p a b", b=4)
            p64 = p6.rearrange("p (a b) -> p a b", b=6)
            pm4 = pm.rearrange("p (a b) -> p a b", b=4)
            ops = [
                lambda e: e.scalar_tensor_tensor(out=lg, in0=g.ps[RB][:, 0:NE], scalar=1.0 / ALPHA, in1=pvs(g, "b_router"), op0=ALU.mult, op1=ALU.add),
                lambda e: e.tensor_reduce(out=mx, in_=lg, axis=AX.X, op=ALU.max, negate=True),
            ]
            s.op("dve", ops, rd=[("ps", RB), "pv"], wr=[rk])
            s.op("act", lambda e: e.activation(out=ex, in_=lg, func=AF.Exp, bias=mx), rd=[rk], wr=[rk])
            ops = [
                lambda e: e.tensor_tensor(out=p64[:, :, 0:3], in0=ex4[:, :, 0:3], in1=ex4[:, :, 1:4], op=ALU.add),
                lambda e: e.tensor_tensor(out=p64[:, :, 3:5], in0=ex4[:, :, 0:2], in1=ex4[:, :, 2:4], op=ALU.add),
                lambda e: e.tensor_tensor(out=p64[:, :, 5:6], in0=ex4[:, :, 0:1], in1=ex4[:, :, 3:4], op=ALU.add),
                lambda e: e.tensor_reduce(out=gs, in_=p64, axis=AX.X, op=ALU.max),
                lambda e: e.tensor_reduce(out=gm, in_=gs, axis=AX.X, op=ALU.max),
                lambda e: e.tensor_scalar(goh, gs, gm, None, ALU.is_ge),
                lambda e: e.tensor_tensor(out=pm4, in0=ex4, in1=goh.unsqueeze(2).to_broadcast([128, 4, 4]), op=ALU.mult),
                lambda e: e.max(out=top8, in_=pm),
                lambda e: e.tensor_scalar(selm, pm, top8[:, 1:2], None, ALU.is_ge),
                lambda e: e.tensor_tensor(out=den, in0=top8[:, 0:1], in1=top8[:, 1:2], op=ALU.add),
                lambda e: e.reciprocal(out=den, in_=den),
                lambda e: e.scalar_tensor_tensor(out=G[:, i, :], in0=pm, scalar=den, in1=selm, op0=ALU.mult, op1=ALU.mult),
            ]
            s.op("dve", ops, rd=[rk], wr=[rk, ("G", i)])

        def prep(t):
            cast_tile(t)
            for i in range(4 * t, 4 * t + 4):
                route_tile(i)

        UB = [0, 1, 2, 3]
        YB = [4, 5]
        GBK = 6
        xbf_res = {t: [("xbf", c, t) for c in range(KC)] for t in range(NT)}

        def slot_of(e_, idx):
            return (e_ * 6 + idx) % RING

        def phaseA(e_, t):
            par = (e_ * NT + t) % 2
            tc = slice(t * TW, (t + 1) * TW)
            for q in range(4):
                i = t * 4 + q
                s.op("dve", lambda e, i=i, q=q: e.tensor_scalar(dg[:, q % 2, :], g.ident[:], G[:, i, e_:e_ + 1], None, ALU.mult),
                     rd=[("G", i), "ident"], wr=[("dg", q % 2)])
                s.op("pe", lambda e, q=q: e.matmul(g.ps[GBK][:, q * 128:(q + 1) * 128], lhsT=g.ones1[:], rhs=dg[:, q % 2, :], start=True, stop=True),
                     rd=[("dg", q % 2), "ones1"], wr=[("ps", GBK)])
            s.op("act", lambda e: e.activation(out=gbs[:, par, :], in_=g.ps[GBK][:], func=AF.Copy), rd=[("ps", GBK)], wr=[("gbs", par)])
            for m in range(KC):
                half, lc = m // 4, (m % 4) * 128
                sl1, sl3 = slot_of(e_, 2 * half), slot_of(e_, 2 * half + 1)
                b1 = next_bank(g, "moeU", UB)
                b3 = next_bank(g, "moeU", UB)
                mm_group(g, g.ps[b1][:], [(ring[:, sl1, k, lc:lc + 128], xbf[:, k, tc]) for k in range(KC)],
                         rd=[("ring", sl1)] + xbf_res[t], wr=[("ps", b1)])
                mm_group(g, g.ps[b3][:], [(ring[:, sl3, k, lc:lc + 128], xbf[:, k, tc]) for k in range(KC)],
                         rd=[("ring", sl3)] + xbf_res[t], wr=[("ps", b3)])
                s.op("act", lambda e, b1=b1, m=m: e.activation(out=s1[:, m % 2, :], in_=g.ps[b1][:], func=AF.Silu),
                     rd=[("ps", b1)], wr=[("s1", m % 2)])
                s.op("dve", lambda e, b3=b3, m=m: e.tensor_tensor(out=s2[:, m % 2, :], in0=g.ps[b3][:], in1=s1[:, m % 2, :], op=ALU.mult),
                     rd=[("ps", b3), ("s1", m % 2)], wr=[("s2", m % 2)])
                s.op("dve", lambda e, m=m: e.tensor_tensor(out=hb[:, par, m, :], in0=s2[:, m % 2, :], in1=gbs[:, par, :], op=ALU.mult),
                     rd=[("s2", m % 2), ("gbs", par)], wr=[("hb", par, m)])
                if t == NT - 1 and m % 4 == 3:
                    issue_piece()
                    issue_piece()

        def phaseB(e_, t):
            par = (e_ * NT + t) % 2
            tc = slice(t * TW, (t + 1) * TW)
            for n in range(KC):
                half, lc = n // 4, (n % 4) * 128
                sl2 = slot_of(e_, 4 + half)
                by = next_bank(g, "moeY", YB)
                mm_group(g, g.ps[by][:], [(ring[:, sl2, m, lc:lc + 128], hb[:, par, m, :]) for m in range(KC)],
                         rd=[("ring", sl2)] + [("hb", par, m) for m in range(KC)], wr=[("ps", by)])
                s.op("dve", lambda e, by=by, n=n: e.tensor_tensor(out=g.xres[:, n, tc], in0=g.ps[by][:], in1=g.xres[:, n, tc], op=ALU.add),
                     rd=[("ps", by), ("xres", n, t)], wr=[("xres", n, t)])
                if t == NT - 1 and n % 4 == 3:
                    issue_piece()

        def ln2_tile(t):
            tc = slice(t * TW, (t + 1) * TW)
            if final:
                of = lambda c, tc=tc, t=t: [(g.xres[:, c, tc], ("xres", c, t), pvs(g, "ln2_g%d" % layer, c), pvs(g, "ln2_b%d" % layer, c))]
            else:
                of = lambda c, tc=tc, t=t: [(g.xres[:, c, tc], ("xres", c, t), g.pvx[:, 16 + c:17 + c], g.pvx[:, 24 + c:25 + c])]
            layer_norm_tile(g, t, KC, lambda c, tc=tc: g.xres[:, c, tc], lambda c, t=t: ("xres", c, t), g.onesD, "onesD", of, tmp, (7, 6))

        steps = [(e_, t) for e_ in range(NE) for t in range(NT)]
        for i, (e_, t) in enumerate(steps):
            if e_ == 0:
                prep(t)
            phaseA(e_, t)
            if i > 0:
                pe_, pt_ = steps[i - 1]
                phaseB(pe_, pt_)
                if pe_ == NE - 1:
                    ln2_tile(pt_)
        phaseB(*steps[-1])
        ln2_tile(NT - 1)


def write_out(g, st0):
    nc, s = g.nc, g.s
    with ExitStack() as st:
        ot = st.enter_context(g.sbuf("ot", [128, 2, D], F32))
        outv = g.out.rearrange("(i p) d -> i p d", p=128)
        OB = [0, 1, 2, 3]
        for i in range(16):
            t = i // 4
            for hf in range(2):
                b = next_bank(g, "wo", OB)
                s.op("pe", [lambda e, b=b, q=q, hf=hf, i=i: e.transpose(g.ps[b][:, q * 128:(q + 1) * 128], g.xres[:, hf * 4 + q, i * 128:(i + 1) * 128], g.ident[:])
                            for q in range(4)], rd=[("xres", hf * 4 + q, t) for q in range(4)] + ["ident"], wr=[("ps", b)])
                if hf == 0:
                    s.op("act", lambda e, b=b, i=i: e.activation(out=ot[:, i % 2, 0:512], in_=g.ps[b][:], func=AF.Copy), rd=[("ps", b)], wr=[("ot", i % 2, 0)])
                else:
                    s.op("dve", lambda e, b=b, i=i: e.tensor_copy(out=ot[:, i % 2, 512:1024], in_=g.ps[b][:]), rd=[("ps", b)], wr=[("ot", i % 2, 1)])
            s.dma("sp", lambda e, i=i: e.dma_start(out=outv[i], in_=ot[:, i % 2, :]), ("out", i % 2), rd=[("ot", i % 2, 0), ("ot", i % 2, 1)],
                  wr=[("outdram", i)])
        s.wait_res("sp", [("outdram", i) for i in range(16)])


_PROG_CACHE = {}


def make_in_maps(inp, stage="full", hp_all=None):
    x = np.asarray(inp["x"], np.float32)
    maps = []
    shared = {
        "even_w_in": np.ascontiguousarray(inp["even_w_in"][0]),
        "even_w_out": np.ascontiguousarray(inp["even_w_out"][0]),
        "odd_w_in": np.ascontiguousarray(inp["odd_w_in"][0]),
        "odd_w_gate_a": np.ascontiguousarray(inp["odd_w_gate_a"][0]),
        "odd_w_gate_x": np.ascontiguousarray(inp["odd_w_gate_x"][0]),
        "odd_w_out": np.ascontiguousarray(inp["odd_w_out"][0]),
        "w_router": np.ascontiguousarray(inp["w_router"]),
    }
    for l in range(DEPTH):
        if (l == 0 and stage in ("full", "l0", "fused")) or (l == 1 and stage in ("full", "l1f", "fused")):
            for nm in ("moe_w1", "moe_w3", "moe_w2"):
                shared["%s_%d" % (nm, l)] = np.ascontiguousarray(inp[nm][l])
    if stage in ("l1f", "l1mix"):
        shared["hp_all"] = np.ascontiguousarray(hp_all, dtype=np.float32)
    for core in range(NCORES):
        b, q = core // 4, core % 4
        if stage == "fused":
            xin = np.zeros((4 * T + HALO, D), np.float32)
            n = (q + 1) * T
            xin[HALO + 4 * T - n:] = x[b, :n]
        else:
            xin = np.zeros((T + HALO, D), np.float32)
            xin[HALO:] = x[b, q * T:(q + 1) * T]
            if q > 0:
                xin[:HALO] = x[b, q * T - HALO:q * T]
        m = dict(shared)
        m["xin"] = xin
        m["pvec"] = pack_pvec(inp, core)
        maps.append(m)
    return maps


def _prog(stage):
    if stage not in _PROG_CACHE:
        _PROG_CACHE[stage] = build_program(stage)
    return _PROG_CACHE[stage]


def kernel(**inputs):

    inp = {k: np.asarray(v) for k, v in inputs.items()}
    cores = list(range(NCORES))
    r0 = run_bass_kernel_spmd(_prog("l0"), make_in_maps(inp, "l0"), core_ids=cores)
    x1 = np.stack([r0.results[c]["out"] for c in cores], axis=0).reshape(2, 4 * T, D)
    inp1 = dict(inp)
    inp1["x"] = x1
    r1 = run_bass_kernel_spmd(_prog("l1s"), make_in_maps(inp1, "l1s"), core_ids=cores)
    hp_all = np.ascontiguousarray(np.stack([r1.results[c]["hp"] for c in cores], axis=1))
    r2 = run_bass_kernel_spmd(_prog("l1f"), make_in_maps(inp1, "l1f", hp_all), core_ids=cores)
    out = np.stack([r2.results[c]["out"] for c in cores], axis=0)
    return out.reshape(2, 4 * T, D).astype(np.float32)


def kernel_fused(**inputs):
    inp = {k: np.asarray(v) for k, v in inputs.items()}
    cores = list(range(NCORES))
    r = run_bass_kernel_spmd(_prog("fused"), make_in_maps(inp, "fused"), core_ids=cores)
    out = np.stack([r.results[c]["out"] for c in cores], axis=0)
    return out.reshape(2, 4 * T, D).astype(np.float32)
```
